# Optimizing a Trainium2 kernel written in Bass

```python
import math
import jax, jax.numpy as jnp
from jax import lax
import numpy as np

D_MODEL = 1024
BATCH = 16
SEQ = 4096
DEPTH = 1

PLE_DIM = 256
SSM_WIDTH = D_MODEL // 2
SSM_GROUP = 16
SSM_GROUPS = SSM_WIDTH // SSM_GROUP
SSM_STATE = 64
POOL_WIDTH = D_MODEL // 2
POOL_WINDOWS = (2, 4, 8, 16)
POOL_GROUPS = len(POOL_WINDOWS)
POOL_GROUP_DIM = POOL_WIDTH // POOL_GROUPS
N_BRANCHES = 2
IN_PROJ_WIDTH = SSM_WIDTH + POOL_WIDTH + N_BRANCHES * D_MODEL
N_EXPERTS = 32
TOP_K = 4
D_EXPERT = D_MODEL
SWIGLU_LIMIT = 7.0
SWIGLU_ALPHA = 1.702
EXPERT_BLOCK = 128
LN_EPS = 1e-5
DEEPNORM_ALPHA = (2.0 * DEPTH) ** 0.25
DEEPNORM_BETA = (8.0 * DEPTH) ** -0.25

kernel_name = "hybrid_s5_pool_moe_deepnorm"


def _layer_norm(x, g, b):
    xf = x.astype(jnp.float32)
    mu = jnp.mean(xf, axis=-1, keepdims=True)
    xc = xf - mu
    var = jnp.mean(xc * xc, axis=-1, keepdims=True)
    y = xc * lax.rsqrt(var + LN_EPS)
    return (y * g.astype(jnp.float32) + b.astype(jnp.float32)).astype(x.dtype)


def _linear_recurrence_op(e1, e2):
    a1, b1 = e1
    a2, b2 = e2
    return a2 * a1, a2 * b1 + b2


def _s5_branch(u, lam_re, lam_im, log_step, b_re, b_im, c_re, c_im, d_skip, w_val, w_gate):
    bsz, seq, _ = u.shape
    f32 = jnp.float32
    uf = u.astype(f32).reshape(bsz, seq, SSM_GROUPS, SSM_GROUP)
    lam = lax.complex(lam_re.astype(f32), lam_im.astype(f32))
    step = jnp.exp(log_step.astype(f32))[:, None]
    lam_bar = jnp.exp(lam * step)
    b_cplx = lax.complex(b_re.astype(f32), b_im.astype(f32))
    b_bar = ((lam_bar - 1.0) / lam)[..., None] * b_cplx
    bu = jnp.einsum('blgh,gph->blgp', uf.astype(jnp.complex64), b_bar)
    a = jnp.broadcast_to(lam_bar, (1, seq) + lam_bar.shape)
    _, states = lax.associative_scan(_linear_recurrence_op, (a, bu), axis=1)
    c_cplx = lax.complex(c_re.astype(f32), c_im.astype(f32))
    y = jnp.einsum('blgp,ghp->blgh', states, c_cplx).real + d_skip.astype(f32) * uf
    z = jax.nn.gelu(y.reshape(bsz, seq, SSM_WIDTH))
    return (z @ w_val) * jax.nn.sigmoid(z @ w_gate)


def _pool_branch(u, w_group, scale, w_proj):
    bsz, seq, _ = u.shape
    f32 = jnp.float32
    uf = u.astype(f32).reshape(bsz, seq, POOL_GROUPS, POOL_GROUP_DIM)
    cs = jnp.cumsum(uf, axis=1)
    pos = jnp.arange(1, seq + 1, dtype=f32)
    outs = []
    for g, w in enumerate(POOL_WINDOWS):
        cs_g = cs[:, :, g]
        lagged = jnp.pad(cs_g[:, :seq - w], ((0, 0), (w, 0), (0, 0)))
        count = jnp.minimum(pos, float(w))[None, :, None]
        outs.append((cs_g - lagged) / count - uf[:, :, g])
    pooled = jnp.stack(outs, axis=2)
    mixed = jnp.einsum('blgc,gcd->blgd', pooled, w_group.astype(f32)).reshape(bsz, seq, POOL_WIDTH)
    return (mixed * scale) @ w_proj


def _moe(h, w_router, b_router, w_gate, b_gate, w_up, b_up, w_down, b_down):
    bsz, seq, d = h.shape
    t = h.reshape(-1, d)
    n_tok = t.shape[0]
    logits = (t @ w_router + b_router).astype(jnp.float32)
    top_val, top_idx = lax.top_k(logits, TOP_K)
    weights = jax.nn.softmax(top_val, axis=-1)
    n_slots = n_tok * TOP_K
    n_blocks = -(-n_slots // EXPERT_BLOCK) + N_EXPERTS
    n_rows = n_blocks * EXPERT_BLOCK
    flat_e = top_idx.reshape(-1).astype(jnp.int32)
    order = jnp.argsort(flat_e)
    sorted_e = flat_e[order]
    counts = jnp.bincount(flat_e, length=N_EXPERTS)
    start = jnp.cumsum(counts) - counts
    padded = ((counts + EXPERT_BLOCK - 1) // EXPERT_BLOCK) * EXPERT_BLOCK
    pad_end = jnp.cumsum(padded)
    pad_start = pad_end - padded
    rank = jnp.arange(n_slots, dtype=jnp.int32) - start[sorted_e]
    dest_sorted = (pad_start[sorted_e] + rank).astype(jnp.int32)
    row_token = jnp.full((n_rows,), n_tok, jnp.int32).at[dest_sorted].set((order // TOP_K).astype(jnp.int32))
    t_pad = jnp.concatenate([t, jnp.zeros((1, d), t.dtype)], axis=0)
    xs = t_pad[row_token].reshape(n_blocks, EXPERT_BLOCK, d)
    block_start = jnp.arange(n_blocks, dtype=pad_end.dtype) * EXPERT_BLOCK
    block_expert = jnp.minimum(jnp.searchsorted(pad_end, block_start, side='right'), N_EXPERTS - 1)

    def expert_block(args):
        xb, e = args
        g = xb @ w_gate[e] + b_gate[e]
        u = xb @ w_up[e] + b_up[e]
        g = jnp.minimum(g, SWIGLU_LIMIT)
        u = jnp.clip(u, -SWIGLU_LIMIT, SWIGLU_LIMIT)
        glu = g * jax.nn.sigmoid(SWIGLU_ALPHA * g)
        return ((u + 1.0) * glu) @ w_down[e] + b_down[e]

    ys = lax.map(expert_block, (xs, block_expert)).reshape(n_rows, d)
    dest = jnp.zeros((n_slots,), jnp.int32).at[order].set(dest_sorted).reshape(n_tok, TOP_K)
    out = jnp.einsum('tk,tkd->td', weights.astype(ys.dtype), ys[dest])
    return out.reshape(bsz, seq, d)


def setup_inputs(seed: int = 0) -> dict:
    key = jax.random.key(seed)
    ks = iter(jax.random.split(key, 40))
    f32 = jnp.float32

    def nrm(shape, scale):
        return jax.random.normal(next(ks), shape, f32) * scale

    L = DEPTH
    D = D_MODEL
    G, P, H = SSM_GROUPS, SSM_STATE, SSM_GROUP
    n_idx = jnp.arange(P, dtype=f32)
    return {
        "x": nrm((BATCH, SEQ, D), 1.0),
        "p": nrm((DEPTH, BATCH, SEQ, PLE_DIM), 1.0),
        "w_in": nrm((L, D, IN_PROJ_WIDTH), D ** -0.5),
        "ssm_lambda_re": -0.5 + nrm((L, G, P), 0.01),
        "ssm_lambda_im": math.pi * n_idx + nrm((L, G, P), 0.01),
        "ssm_log_step": jax.random.uniform(next(ks), (L, G), f32, math.log(1e-3), math.log(1e-1)),
        "ssm_b_re": nrm((L, G, P, H), (2.0 * H) ** -0.5),
        "ssm_b_im": nrm((L, G, P, H), (2.0 * H) ** -0.5),
        "ssm_c_re": nrm((L, G, H, P), (2.0 * P) ** -0.5),
        "ssm_c_im": nrm((L, G, H, P), (2.0 * P) ** -0.5),
        "ssm_d": nrm((L, G, H), 1.0),
        "w_glu_val": nrm((L, SSM_WIDTH, D), SSM_WIDTH ** -0.5),
        "w_glu_gate": nrm((L, SSM_WIDTH, D), SSM_WIDTH ** -0.5),
        "w_pool_group": nrm((L, POOL_GROUPS, POOL_GROUP_DIM, POOL_GROUP_DIM), POOL_GROUP_DIM ** -0.5),
        "pool_scale": 1.0 + nrm((L, POOL_WIDTH), 0.02),
        "w_pool_proj": nrm((L, POOL_WIDTH, D), POOL_WIDTH ** -0.5),
        "w_out": nrm((L, D, D), DEEPNORM_BETA * D ** -0.5),
        "ln1_g": 1.0 + nrm((L, D), 0.02),
        "ln1_b": nrm((L, D), 0.02),
        "w_router": nrm((L, D, N_EXPERTS), D ** -0.5),
        "b_router": nrm((L, N_EXPERTS), 0.01),
        "w_gate": nrm((L, N_EXPERTS, D, D_EXPERT), D ** -0.5),
        "b_gate": nrm((L, N_EXPERTS, D_EXPERT), 0.01),
        "w_up": nrm((L, N_EXPERTS, D, D_EXPERT), D ** -0.5),
        "b_up": nrm((L, N_EXPERTS, D_EXPERT), 0.01),
        "w_down": nrm((L, N_EXPERTS, D_EXPERT, D), DEEPNORM_BETA * D_EXPERT ** -0.5),
        "b_down": nrm((L, N_EXPERTS, D), 0.01),
        "w_ple_gate": nrm((L, D, D), D ** -0.5),
        "w_ple_proj": nrm((L, PLE_DIM, D), PLE_DIM ** -0.5),
        "ln2_g": 1.0 + nrm((L, D), 0.02),
        "ln2_b": nrm((L, D), 0.02),
    }


def reference(x, p, w_in, ssm_lambda_re, ssm_lambda_im, ssm_log_step, ssm_b_re, ssm_b_im,
              ssm_c_re, ssm_c_im, ssm_d, w_glu_val, w_glu_gate, w_pool_group, pool_scale,
              w_pool_proj, w_out, ln1_g, ln1_b, w_router, b_router, w_gate, b_gate, w_up, b_up,
              w_down, b_down, w_ple_gate, w_ple_proj, ln2_g, ln2_b):
    h = x
    for i in range(DEPTH):
        proj = h @ w_in[i]
        u_ssm = proj[..., :SSM_WIDTH]
        u_pool = proj[..., SSM_WIDTH:SSM_WIDTH + POOL_WIDTH]
        gate_a = jax.nn.sigmoid(proj[..., SSM_WIDTH + POOL_WIDTH:SSM_WIDTH + POOL_WIDTH + D_MODEL])
        gate_b = jax.nn.sigmoid(proj[..., SSM_WIDTH + POOL_WIDTH + D_MODEL:])
        y_ssm = _s5_branch(u_ssm, ssm_lambda_re[i], ssm_lambda_im[i], ssm_log_step[i],
                           ssm_b_re[i], ssm_b_im[i], ssm_c_re[i], ssm_c_im[i], ssm_d[i],
                           w_glu_val[i], w_glu_gate[i])
        y_pool = _pool_branch(u_pool, w_pool_group[i], pool_scale[i], w_pool_proj[i])
        merged = gate_a * y_ssm + gate_b * y_pool
        h = _layer_norm(DEEPNORM_ALPHA * h + merged @ w_out[i], ln1_g[i], ln1_b[i])
        moe_out = _moe(h, w_router[i], b_router[i], w_gate[i], b_gate[i], w_up[i], b_up[i],
                       w_down[i], b_down[i])
        ple = jax.nn.sigmoid(h @ w_ple_gate[i]) * (p[i] @ w_ple_proj[i])
        h = _layer_norm(DEEPNORM_ALPHA * h + moe_out + ple, ln2_g[i], ln2_b[i])
    return h
```

```python
import types
import numpy as np
from contextlib import ExitStack
import concourse.bass as bass
import concourse.mybir as mybir
from concourse.bass_utils import run_bass_kernel_spmd

F32 = mybir.dt.float32
BF16 = mybir.dt.bfloat16
I32 = mybir.dt.int32
AF = mybir.ActivationFunctionType
ALU = mybir.AluOpType
AX = mybir.AxisListType

ENGS = ["sync", "act", "dve", "pool", "pe"]
NT = 512
ALPHA = float(2.0 ** 0.25)
TWO_PI = float(2 * np.pi)


def _snap(fn):
    if fn is None or fn.__closure__ is None:
        return fn
    cells = []
    for c in fn.__closure__:
        try:
            cells.append(types.CellType(c.cell_contents))
        except ValueError:
            cells.append(c)
    g = types.FunctionType(fn.__code__, fn.__globals__, fn.__name__, fn.__defaults__, tuple(cells))
    g.__kwdefaults__ = fn.__kwdefaults__
    return g


class Prog:
    def __init__(self, nc, es):
        self.nc = nc
        self.es = es
        self.ops = {e: [] for e in ENGS}
        self.cnt = {e: 0 for e in ENGS}
        self.esem = {}
        for e in ["act", "dve", "pool", "pe"]:
            self.esem[e] = es.enter_context(nc.semaphore("s_" + e))
        self.dsem = {}
        self.last_w = {}
        self.readers = {}
        self.seen = {e: {} for e in ENGS}

    def _need(self, eng, ev, waits):
        if ev is None:
            return
        sem, val, src = ev
        if src == "pe" and eng == "pe":
            return
        k = id(sem)
        cur = self.seen[eng].get(k, (None, 0))[1]
        if cur >= val:
            return
        self.seen[eng][k] = (sem, val)
        waits[k] = (sem, max(val, waits.get(k, (None, 0))[1]))

    def _deps(self, eng, reads, writes):
        waits = {}
        for k in reads:
            self._need(eng, self.last_w.get(k), waits)
        for k in writes:
            self._need(eng, self.last_w.get(k), waits)
            for r in self.readers.get(k, []):
                self._need(eng, r, waits)
        return list(waits.values())

    def _commit(self, ev, reads, writes):
        for k in reads:
            self.readers.setdefault(k, []).append(ev)
        for k in writes:
            self.last_w[k] = ev
            self.readers[k] = []

    def op(self, eng, fn, reads=(), writes=()):
        waits = self._deps(eng, reads, writes)
        self.cnt[eng] += 1
        ev = (self.esem[eng], self.cnt[eng], eng)
        self.ops[eng].append((waits, _snap(fn), [(self.esem[eng], 1)]))
        self._commit(ev, reads, writes)

    def dma(self, eng, fns, reads=(), writes=(), key=None):
        if key is None:
            key = writes[0]
        if key not in self.dsem:
            self.dsem[key] = [self.es.enter_context(self.nc.semaphore("d%d" % len(self.dsem))), 0]
        ent = self.dsem[key]
        waits = self._deps(eng, reads, writes)
        for i, fn in enumerate(fns):
            ent[1] += 16
            self.ops[eng].append((waits if i == 0 else [], _snap(fn), [(ent[0], 16)]))
        ev = (ent[0], ent[1], "dma")
        self._commit(ev, reads, writes)

    def seal(self, key, keys):
        ent = self.dsem[key]
        for k in keys:
            self.last_w[k] = (ent[0], ent[1], "dma")

    def barrier(self):
        evs = [(self.esem[x], self.cnt[x], x) for x in ["act", "dve", "pool", "pe"] if self.cnt[x] > 0]
        evs += [(ent[0], ent[1], "dma") for k_, ent in self.dsem.items() if ent[1] > 0 and not str(k_).startswith("prep")]
        for eng in ENGS:
            waits = {}
            for sem, val, src in evs:
                kk = id(sem)
                if self.seen[eng].get(kk, (None, 0))[1] >= val:
                    continue
                self.seen[eng][kk] = (sem, val)
                waits[kk] = (sem, val)
            if waits:
                self.ops[eng].append((list(waits.values()), None, []))

    def final_wait(self, eng, keys):
        waits = self._deps(eng, keys, [])
        self.ops[eng].append((waits, None, []))

    def emit(self, block):
        def run(e, lst):
            for waits, fn, incs in lst:
                for sem, val in waits:
                    e.wait_ge(sem, val)
                if fn is None:
                    continue
                ins = fn(e)
                for sem, v in incs:
                    ins.then_inc(sem, v)

        @block.sync
        def _(e):
            run(e, self.ops["sync"])

        @block.scalar
        def _(e):
            run(e, self.ops["act"])

        @block.vector
        def _(e):
            run(e, self.ops["dve"])

        @block.gpsimd
        def _(e):
            run(e, self.ops["pool"])

        @block.tensor
        def _(e):
            run(e, self.ops["pe"])


IN_SPECS = None


def in_specs(ntok):
    return {
        "xT": ([8, 128, ntok], F32), "pT": ([2, 128, ntok], F32),
        "w_in": ([8, 128, 3072], F32), "w_val": ([4, 128, 1024], F32), "w_gg": ([4, 128, 1024], F32),
        "w_pp": ([4, 128, 1024], F32), "w_out": ([8, 128, 1024], F32),
        "w_plg": ([8, 128, 1024], F32), "w_plp": ([2, 128, 1024], F32),
        "w_pg": ([128, 4, 128], F32), "pscale": ([128, 4], F32),
        "ln1g": ([128, 8], F32), "ln1b": ([128, 8], F32), "ln2g": ([128, 8], F32), "ln2b": ([128, 8], F32),
        "w_r": ([128, 8, 32], F32), "b_r": ([128, 32], F32),
        "w_eg": ([32, 8, 128, 1024], F32), "w_eu": ([32, 8, 128, 1024], F32), "w_ed": ([32, 8, 128, 1024], F32),
        "b_eg": ([128, 32, 8], F32), "b_eu": ([128, 32, 8], F32),
        "lrS": ([128, 16], F32), "liS": ([128, 16], F32), "lsS": ([128, 16], F32),
        "cSr": ([128, 16, 32], F32), "cSi": ([128, 16, 32], F32),
        "bSr": ([128, 16, 32], F32), "bSi": ([128, 16, 32], F32),
        "lrT": ([128, 4, 128], F32), "liT": ([128, 4, 128], F32), "lsT": ([128, 4, 128], F32),
        "bTr": ([128, 4, 128], F32), "bTi": ([128, 4, 128], F32), "dT": ([128, 4], F32),
        "ident": ([128, 128], F32), "invc": ([128, 4, 16], F32), "tri": ([128, 128], F32), "eoff1": ([128, 32], F32),
        "b_down": ([32, 1024], F32),
    }


def build(nseq, seqlen, n_experts=32, dbg=False, cap=1536):
    assert nseq == 2
    ntok = nseq * seqlen
    ntiles = ntok // NT
    HS = NT // 2
    nc = bass.Bass("TRN2", target_bir_lowering=False)
    D = {}
    for name, (shape, dt) in in_specs(ntok).items():
        D[name] = nc.dram_tensor(name, shape, dt, kind="ExternalInput").ap()
    outT = nc.dram_tensor("outT", [8, 128, ntok], F32, kind="ExternalOutput").ap()
    h1T = nc.dram_tensor("h1T", [8, 128, ntok], F32, kind="ExternalOutput" if dbg else "Internal").ap()
    NSLOT = 32 * cap
    NBLK = cap // 128
    NBLKT = ntok // 128
    h1tm16 = nc.dram_tensor("h1tm16", [ntok + 1, 1024], BF16, kind="Internal").ap()
    wtab = nc.dram_tensor("wtab", [ntok + 1, 32], F32, kind="Internal").ap()
    toklist = nc.dram_tensor("toklist", [NSLOT, 1], I32, kind="Internal").ap()
    ysd = nc.dram_tensor("ysd", [NSLOT + 1, 1024], F32, kind="Internal").ap()
    S = {}
    for name in ["w_in", "w_val", "w_gg", "w_pp", "w_out", "w_plg", "w_plp", "w_eg", "w_eu", "w_ed"]:
        S[name] = nc.dram_tensor("s_" + name, in_specs(ntok)[name][0], BF16, kind="Internal").ap()

    with ExitStack() as es:
        p = Prog(nc, es)
        T = lambda n, s, d, st=es: st.enter_context(nc.sbuf_tensor(n, s, d))
        banks = [es.enter_context(nc.psum_tensor("pb%d" % i, [128, 512], F32)) for i in range(8)]
        block = es.enter_context(nc.Block())
        bank_i = [0]

        def nb(nrot=6):
            i = bank_i[0] % nrot
            bank_i[0] += 1
            return banks[i], "pb%d" % i

        dbg_outs = {}
        bc_regs = {}

        def bcr(e, value):
            if value not in bc_regs:
                r = e.alloc_register("bc%d" % len(bc_regs))
                e.reg_mov(r, int(value))
                bc_regs[value] = r
            return bc_regs[value]

        def dump(name, tile, key, eng="sync"):
            if not dbg or name in dbg_outs:
                return
            shape = list(tile.shape)
            d = nc.dram_tensor("dbg_" + name, shape, tile.dtype, kind="ExternalOutput").ap()
            dbg_outs[name] = d
            p.dma(eng, [lambda e: e.dma_start(out=d, in_=tile[:])], reads=[key], writes=["dbg_" + name], key="dbg")

        fns = []
        for name in ["w_in", "w_val", "w_gg", "w_pp", "w_out", "w_plg", "w_plp"]:
            for kc in range(D[name].shape[0]):
                fns.append(lambda e, name=name, kc=kc: e.dma_start(out=S[name][kc], in_=D[name][kc]))
        p.dma("pool", fns, writes=["prepA"], key="prepA")
        def prep_expert(ex):
            fns = []
            for name in ["w_eg", "w_eu", "w_ed"]:
                for kc in range(8):
                    fns.append(lambda e, name=name, kc=kc, ex=ex: e.dma_start(out=S[name][ex, kc], in_=D[name][ex, kc]))
            p.dma("pool", fns, writes=["prepE%d" % ex], key="prepE%d" % ex)
        prep_per_tile = -(-n_experts // ntiles)
        prep_next = [0]

        cst = {}
        fns = []
        for name in ["pscale", "ln1g", "ln1b", "ln2g", "ln2b", "w_r", "b_r",
                     "dT", "ident", "invc", "eoff1"]:
            shape, dt = in_specs(ntok)[name]
            cst[name] = T("c_" + name, shape, dt)
            fns.append(lambda e, name=name: e.dma_start(out=cst[name][:], in_=D[name]))
        p.dma("sync", fns, writes=["cst"], key="cst")
        p.seal("cst", ["cst"])
        DEST = T("DEST", [128, NBLKT, 4], I32)
        CNT = T("CNT", [128, 32], F32)
        TOKID = T("TOKID", [128, NBLKT], I32)
        TRI = T("TRI", [128, 128], BF16)
        p.op("pool", lambda e: e.iota(TOKID[:], pattern=[[128, NBLKT]], base=0, channel_multiplier=1), writes=["TOKID"])
        p.op("dve", lambda e: e.memset(CNT[:], 0.0), writes=["CNT"])
        with ExitStack() as stw:
            tri32 = T("tri32", [128, 128], F32, stw)
            p.dma("sync", [lambda e: e.dma_start(out=tri32[:], in_=D["tri"])], writes=["tri32"])
            p.op("dve", lambda e: e.tensor_copy(out=TRI[:], in_=tri32[:]), reads=["tri32"], writes=["TRI"])
            p.barrier()
        with ExitStack() as st0:
            z32 = T("z32", [1, 1024], F32, st0); z16 = T("z16", [1, 1024], BF16, st0); tli = T("tli", [128, NSLOT // 128], I32, st0)
            p.op("dve", lambda e: e.memset(z32[:], 0.0), writes=["z32"])
            p.op("dve", lambda e: e.memset(z16[:], 0.0), writes=["z16"])
            p.op("dve", lambda e: e.memset(tli[:], ntok), writes=["tli"])
            p.dma("sync", [lambda e: e.dma_start(out=h1tm16[ntok:ntok + 1, :], in_=z16[:]),
                           lambda e: e.dma_start(out=wtab[ntok:ntok + 1, :], in_=z32[:, 0:32]),
                           lambda e: e.dma_start(out=ysd[NSLOT:NSLOT + 1, :], in_=z32[:]),
                           lambda e: e.dma_start(out=toklist.rearrange("(p a) o -> p (a o)", p=128), in_=tli[:])],
                  reads=["z32", "z16", "tli"], writes=["init"], key="init")
            p.seal("init", ["init"])
            p.barrier()
        identb = T("identb", [128, 128], BF16)
        onesb = T("onesb", [128, 128], BF16)
        wpgb = T("wpgb", [128, 4, 128], BF16)
        p.op("dve", lambda e: e.tensor_copy(out=identb[:], in_=cst["ident"][:]), reads=["cst"], writes=["identb"])
        p.op("dve", lambda e: e.memset(onesb[:], 1.0), writes=["onesb"])
        with ExitStack() as stw:
            wpg32 = T("wpg32", [128, 4, 128], F32, stw)
            p.dma("sync", [lambda e: e.dma_start(out=wpg32[:], in_=D["w_pg"])], writes=["wpg32"])
            p.op("dve", lambda e: e.tensor_copy(out=wpgb[:], in_=wpg32[:]), reads=["wpg32"], writes=["wpgb"])
            p.barrier()

        with ExitStack() as esA:
            TA = lambda n, s, d: T(n, s, d, esA)
            WT = TA("WT", [128, 4, 16, 2, 128], BF16)
            GS = TA("GS", [128, 16, 16, 2, 32], BF16)
            KL = TA("KL", [128, 4, 16, 128], BF16)
            DG = TA("DG", [128, 4, 128], BF16)
            AR = TA("AR", [128, 16], F32)
            AI = TA("AI", [128, 16], F32)

            def powers(pre, tl, lr, li, ls, F, K, st, kpre=None):
                TS = lambda n, s, d: T(pre + n, s, d, st)
                s_ = TS("s", [128, F], F32); a_ = TS("a", [128, F], F32); th = TS("th", [128, F], F32)
                MAG = TS("MAG", [128, K + 1, F], F32); TT = TS("TT", [128, K + 1, F], F32)
                W1 = TS("W1", [128, K + 1, F], F32); I1 = TS("I1", [128, K + 1, F], I32)
                W2 = TS("W2", [128, K + 1, F], F32)
                PWR = TS("PWR", [128, K + 1, F], F32); PWI = TS("PWI", [128, K + 1, F], F32)
                k = lambda n: (kpre or pre) + n
                p.op("act", lambda e: e.activation(out=s_[:], in_=ls, func=AF.Exp), reads=[tl], writes=[k("s")])
                p.op("dve", lambda e: e.tensor_tensor(out=a_[:], in0=lr, in1=s_[:], op=ALU.mult), reads=[tl, k("s")], writes=[k("a")])
                p.op("dve", lambda e: e.tensor_tensor(out=th[:], in0=li, in1=s_[:], op=ALU.mult), reads=[tl, k("s")], writes=[k("th")])
                p.op("dve", lambda e: e.tensor_scalar(out=th[:], in0=th[:], scalar1=float(1.0 / TWO_PI), scalar2=None, op0=ALU.mult), reads=[k("th")], writes=[k("th")])
                for kk in range(K + 1):
                    p.op("act", lambda e, kk=kk: e.activation(out=MAG[:, kk, :], in_=a_[:], func=AF.Exp, scale=float(kk)), reads=[k("a")], writes=[k("MAG")])
                    p.op("dve", lambda e, kk=kk: e.tensor_scalar(out=TT[:, kk, :], in0=th[:], scalar1=float(kk), scalar2=None, op0=ALU.mult), reads=[k("th")], writes=[k("TT")])
                for off, OUT, on in [(0.25, PWR, "PWR"), (0.0, PWI, "PWI")]:
                    p.op("dve", lambda e, off=off: e.tensor_scalar(out=W1[:], in0=TT[:], scalar1=float(off), scalar2=None, op0=ALU.add), reads=[k("TT")], writes=[k("W1")])
                    p.op("dve", lambda e: e.tensor_copy(out=I1[:], in_=W1[:]), reads=[k("W1")], writes=[k("I1")])
                    p.op("dve", lambda e: e.tensor_copy(out=W2[:], in_=I1[:]), reads=[k("I1")], writes=[k("W2")])
                    p.op("dve", lambda e: e.tensor_sub(out=W1[:], in0=W1[:], in1=W2[:]), reads=[k("W1"), k("W2")], writes=[k("W1")])
                    p.op("dve", lambda e: e.tensor_single_scalar(out=W2[:], in_=W1[:], scalar=0.5, op=ALU.is_gt), reads=[k("W1")], writes=[k("W2")])
                    p.op("dve", lambda e: e.tensor_sub(out=W1[:], in0=W1[:], in1=W2[:]), reads=[k("W1"), k("W2")], writes=[k("W1")])
                    p.op("dve", lambda e: e.tensor_single_scalar(out=W2[:], in_=W1[:], scalar=-0.5, op=ALU.is_lt), reads=[k("W1")], writes=[k("W2")])
                    p.op("dve", lambda e: e.tensor_add(out=W1[:], in0=W1[:], in1=W2[:]), reads=[k("W1"), k("W2")], writes=[k("W1")])
                    p.op("act", lambda e, OUT=OUT: e.activation(out=OUT[:], in_=W1[:], func=AF.Sin, scale=TWO_PI), reads=[k("W1")], writes=[k(on)])
                    p.op("dve", lambda e, OUT=OUT: e.tensor_mul(out=OUT[:], in0=OUT[:], in1=MAG[:]), reads=[k(on), k("MAG")], writes=[k(on)])
                n1 = TS("n1", [128, F], F32); n2 = TS("n2", [128, F], F32); n3 = TS("n3", [128, F], F32)
                cr = TS("cr", [128, F], F32); ci = TS("ci", [128, F], F32)
                p.op("dve", lambda e: e.tensor_scalar(out=n1[:], in0=PWR[:, 1, :], scalar1=-1.0, scalar2=None, op0=ALU.add), reads=[k("PWR")], writes=[k("n1")])
                p.op("dve", lambda e: e.tensor_mul(out=n2[:], in0=n1[:], in1=lr), reads=[k("n1"), tl], writes=[k("n2")])
                p.op("dve", lambda e: e.tensor_mul(out=n3[:], in0=PWI[:, 1, :], in1=li), reads=[k("PWI"), tl], writes=[k("n3")])
                p.op("dve", lambda e: e.tensor_add(out=cr[:], in0=n2[:], in1=n3[:]), reads=[k("n2"), k("n3")], writes=[k("cr")])
                p.op("dve", lambda e: e.tensor_mul(out=n2[:], in0=PWI[:, 1, :], in1=lr), reads=[k("PWI"), tl, k("cr")], writes=[k("n2")])
                p.op("dve", lambda e: e.tensor_mul(out=n3[:], in0=n1[:], in1=li), reads=[k("n1"), tl, k("cr")], writes=[k("n3")])
                p.op("dve", lambda e: e.tensor_sub(out=ci[:], in0=n2[:], in1=n3[:]), reads=[k("n2"), k("n3")], writes=[k("ci")])
                p.op("dve", lambda e: e.tensor_mul(out=n2[:], in0=lr, in1=lr), reads=[tl, k("ci")], writes=[k("n2")])
                p.op("dve", lambda e: e.tensor_mul(out=n3[:], in0=li, in1=li), reads=[tl, k("ci")], writes=[k("n3")])
                p.op("dve", lambda e: e.tensor_add(out=n2[:], in0=n2[:], in1=n3[:]), reads=[k("n2"), k("n3")], writes=[k("n2")])
                p.op("dve", lambda e: e.reciprocal(out=n2[:], in_=n2[:]), reads=[k("n2")], writes=[k("n2")])
                p.op("dve", lambda e: e.tensor_mul(out=cr[:], in0=cr[:], in1=n2[:]), reads=[k("cr"), k("n2")], writes=[k("cr")])
                p.op("dve", lambda e: e.tensor_mul(out=ci[:], in0=ci[:], in1=n2[:]), reads=[k("ci"), k("n2")], writes=[k("ci")])
                return PWR, PWI, cr, ci

            for ch in range(4):
                with ExitStack() as st:
                    pre = "T%d_" % ch
                    TS = lambda n, s, d: T(pre + n, s, d, st)
                    inT = TS("in", [128, 5, 128], F32)
                    p.dma("sync", [lambda e, i=i, nm=nm, ch=ch: e.dma_start(out=inT[:, i, :], in_=D[nm][:, ch, :])
                                   for i, nm in enumerate(["lrT", "liT", "lsT", "bTr", "bTi"])], writes=["T_in"], key="ldT")
                    PWR, PWI, cr, ci = powers(pre, "T_in", inT[:, 0, :], inT[:, 1, :], inT[:, 2, :], 128, 15, st, "T_")
                    bbr = TS("bbr", [128, 128], F32); bbi = TS("bbi", [128, 128], F32)
                    t1 = TS("t1", [128, 16, 128], F32); t2 = TS("t2", [128, 16, 128], F32)
                    k = lambda n: "T_" + n
                    p.op("dve", lambda e: e.tensor_mul(out=t1[:, 0, :], in0=cr[:], in1=inT[:, 3, :]), reads=[k("cr"), k("in")], writes=[k("t1")])
                    p.op("dve", lambda e: e.tensor_mul(out=t2[:, 0, :], in0=ci[:], in1=inT[:, 4, :]), reads=[k("ci"), k("in")], writes=[k("t2")])
                    p.op("dve", lambda e: e.tensor_sub(out=bbr[:], in0=t1[:, 0, :], in1=t2[:, 0, :]), reads=[k("t1"), k("t2")], writes=[k("bbr")])
                    p.op("dve", lambda e: e.tensor_mul(out=t1[:, 0, :], in0=cr[:], in1=inT[:, 4, :]), reads=[k("cr"), k("in"), k("bbr")], writes=[k("t1")])
                    p.op("dve", lambda e: e.tensor_mul(out=t2[:, 0, :], in0=ci[:], in1=inT[:, 3, :]), reads=[k("ci"), k("in"), k("bbr")], writes=[k("t2")])
                    p.op("dve", lambda e: e.tensor_add(out=bbi[:], in0=t1[:, 0, :], in1=t2[:, 0, :]), reads=[k("t1"), k("t2")], writes=[k("bbi")])
                    bc = lambda t: t[:].unsqueeze(1).to_broadcast([128, 16, 128])
                    p.op("dve", lambda e: e.tensor_tensor(out=t1[:], in0=PWR[:], in1=bc(bbr), op=ALU.mult), reads=[k("PWR"), k("bbr"), k("bbi")], writes=[k("t1")])
                    p.op("dve", lambda e: e.tensor_tensor(out=t2[:], in0=PWI[:], in1=bc(bbi), op=ALU.mult), reads=[k("PWI"), k("bbi")], writes=[k("t2")])
                    p.op("dve", lambda e, ch=ch: e.tensor_sub(out=WT[:, ch, :, 0, :], in0=t1[:], in1=t2[:]), reads=[k("t1"), k("t2")], writes=["WT"])
                    p.op("dve", lambda e: e.tensor_tensor(out=t1[:], in0=PWR[:], in1=bc(bbi), op=ALU.mult), reads=[k("PWR"), k("bbi"), "WT"], writes=[k("t1")])
                    p.op("dve", lambda e: e.tensor_tensor(out=t2[:], in0=PWI[:], in1=bc(bbr), op=ALU.mult), reads=[k("PWI"), k("bbr"), "WT"], writes=[k("t2")])
                    p.op("dve", lambda e, ch=ch: e.tensor_add(out=WT[:, ch, :, 1, :], in0=t1[:], in1=t2[:]), reads=[k("t1"), k("t2")], writes=["WT"])
                    p.op("dve", lambda e, ch=ch: e.tensor_scalar(out=DG[:, ch, :], in0=cst["ident"][:], scalar1=cst["dT"][:, ch:ch + 1], scalar2=None, op0=ALU.mult), reads=["cst"], writes=["DG"])

            p.barrier()
            with ExitStack() as st:
                pre = "S_"
                TS = lambda n, s, d: T(pre + n, s, d, st)
                k = lambda n: pre + n
                inS = TS("in", [128, 3, 16], F32)
                cS = TS("c", [128, 2, 16, 32], F32)
                bS = TS("b", [128, 2, 16, 32], F32)
                p.dma("sync", [lambda e, i=i, nm=nm: e.dma_start(out=inS[:, i, :], in_=D[nm]) for i, nm in enumerate(["lrS", "liS", "lsS"])]
                      + [lambda e, i=i, nm=nm: e.dma_start(out=cS[:, i], in_=D[nm]) for i, nm in enumerate(["cSr", "cSi"])]
                      + [lambda e, i=i, nm=nm: e.dma_start(out=bS[:, i], in_=D[nm]) for i, nm in enumerate(["bSr", "bSi"])],
                      writes=[k("in")], key="ldS")
                p.seal("ldS", [k("in")])
                PWR, PWI, cr, ci = powers(pre, k("in"), inS[:, 0, :], inS[:, 1, :], inS[:, 2, :], 16, 16, st)
                p.op("dve", lambda e: e.tensor_copy(out=AR[:], in_=PWR[:, 16, :]), reads=[k("PWR")], writes=["AR"])
                p.op("dve", lambda e: e.tensor_copy(out=AI[:], in_=PWI[:, 16, :]), reads=[k("PWI")], writes=["AI"])
                bb = TS("bb", [128, 2, 16, 32], F32)
                ncSi = TS("ncSi", [128, 16, 32], F32)
                u1 = TS("u1", [128, 16, 32], F32); u2 = TS("u2", [128, 16, 32], F32)
                bq = lambda t: t.unsqueeze(2).to_broadcast([128, 16, 32])
                p.op("dve", lambda e: e.tensor_tensor(out=u1[:], in0=bS[:, 0], in1=bq(cr[:]), op=ALU.mult), reads=[k("in"), k("cr")], writes=[k("u1")])
                p.op("dve", lambda e: e.tensor_tensor(out=u2[:], in0=bS[:, 1], in1=bq(ci[:]), op=ALU.mult), reads=[k("in"), k("ci")], writes=[k("u2")])
                p.op("dve", lambda e: e.tensor_sub(out=bb[:, 0], in0=u1[:], in1=u2[:]), reads=[k("u1"), k("u2")], writes=[k("bb")])
                p.op("dve", lambda e: e.tensor_tensor(out=u1[:], in0=bS[:, 1], in1=bq(cr[:]), op=ALU.mult), reads=[k("in"), k("cr"), k("bb")], writes=[k("u1")])
                p.op("dve", lambda e: e.tensor_tensor(out=u2[:], in0=bS[:, 0], in1=bq(ci[:]), op=ALU.mult), reads=[k("in"), k("ci"), k("bb")], writes=[k("u2")])
                p.op("dve", lambda e: e.tensor_add(out=bb[:, 1], in0=u1[:], in1=u2[:]), reads=[k("u1"), k("u2")], writes=[k("bb")])
                p.op("dve", lambda e: e.tensor_scalar(out=ncSi[:], in0=cS[:, 1], scalar1=-1.0, scalar2=None, op0=ALU.mult), reads=[k("in")], writes=[k("ncSi")])
                for j in range(16):
                    pr = lambda j=j: bq(PWR[:, j + 1, :]); pi = lambda j=j: bq(PWI[:, j + 1, :])
                    p.op("dve", lambda e, pr=pr: e.tensor_tensor(out=u1[:], in0=cS[:, 0], in1=pr(), op=ALU.mult), reads=[k("in"), k("PWR"), k("bb"), "GS"], writes=[k("u1")])
                    p.op("dve", lambda e, pi=pi: e.tensor_tensor(out=u2[:], in0=cS[:, 1], in1=pi(), op=ALU.mult), reads=[k("in"), k("PWI"), k("bb"), "GS"], writes=[k("u2")])
                    p.op("dve", lambda e, j=j: e.tensor_sub(out=GS[:, :, j, 0, :], in0=u1[:], in1=u2[:]), reads=[k("u1"), k("u2")], writes=["GS"])
                    p.op("dve", lambda e, pi=pi: e.tensor_tensor(out=u1[:], in0=cS[:, 0], in1=pi(), op=ALU.mult), reads=[k("in"), k("PWI"), "GS"], writes=[k("u1")])
                    p.op("dve", lambda e, pr=pr: e.tensor_tensor(out=u2[:], in0=cS[:, 1], in1=pr(), op=ALU.mult), reads=[k("in"), k("PWR"), "GS"], writes=[k("u2")])
                    p.op("dve", lambda e, j=j: e.scalar_tensor_tensor(out=GS[:, :, j, 1, :], in0=u1[:], scalar=-1.0, in1=u2[:], op0=ALU.mult, op1=ALU.subtract), reads=[k("u1"), k("u2")], writes=["GS"])
                ES = TS("ES", [128, 2, 2, 16, 32], F32)
                p.op("dve", lambda e: e.memset(KL[:], 0.0), writes=["KL"])
                for kk in range(16):
                    sl = kk % 2
                    ek = k("ES%d" % sl)
                    pr = lambda kk=kk: bq(PWR[:, kk, :]); pi = lambda kk=kk: bq(PWI[:, kk, :])
                    p.op("dve", lambda e, pr=pr: e.tensor_tensor(out=u1[:], in0=bb[:, 0], in1=pr(), op=ALU.mult), reads=[k("bb"), k("PWR"), "GS", ek], writes=[k("u1")])
                    p.op("dve", lambda e, pi=pi: e.tensor_tensor(out=u2[:], in0=bb[:, 1], in1=pi(), op=ALU.mult), reads=[k("bb"), k("PWI"), "GS", ek], writes=[k("u2")])
                    p.op("dve", lambda e, sl=sl: e.tensor_sub(out=ES[:, sl, 0], in0=u1[:], in1=u2[:]), reads=[k("u1"), k("u2")], writes=[ek])
                    p.op("dve", lambda e, pi=pi: e.tensor_tensor(out=u1[:], in0=bb[:, 0], in1=pi(), op=ALU.mult), reads=[k("bb"), k("PWI"), ek], writes=[k("u1")])
                    p.op("dve", lambda e, pr=pr: e.tensor_tensor(out=u2[:], in0=bb[:, 1], in1=pr(), op=ALU.mult), reads=[k("bb"), k("PWR"), ek], writes=[k("u2")])
                    p.op("dve", lambda e, sl=sl: e.tensor_add(out=ES[:, sl, 1], in0=u1[:], in1=u2[:]), reads=[k("u1"), k("u2")], writes=[ek])

                    def mmk(e, kk=kk, sl=sl):
                        ins = None
                        for q in range(16):
                            ch, a = q // 4, q % 4
                            o = banks[ch][32 * a:32 * a + 32, kk * 32:(kk + 1) * 32]
                            e.matmul(o, lhsT=ES[:, sl, 0, q, :], rhs=cS[:, 0, q, :], start=True, stop=False,
                                     skip_group_check=True, tile_position=(0, 32 * a))
                            ins = e.matmul(o, lhsT=ES[:, sl, 1, q, :], rhs=ncSi[:, q, :], start=False, stop=True,
                                           skip_group_check=True, tile_position=(0, 32 * a))
                        return ins
                    p.op("pe", mmk, reads=[ek, k("in"), k("ncSi")], writes=["pb0", "pb1", "pb2", "pb3"])
                for ch in range(4):
                    for a in range(4):
                        p.op("dve", lambda e, ch=ch, a=a: e.tensor_copy(
                            out=KL[32 * a:32 * a + 32, ch, :, 32 * a:32 * a + 32],
                            in_=banks[ch][32 * a:32 * a + 32, :].rearrange("p (k c) -> p k c", c=32)),
                            reads=["pb%d" % ch], writes=["KL"])

            p.barrier()
            dump("WT", WT, "WT"); dump("GS", GS, "GS"); dump("KL", KL, "KL"); dump("AR", AR, "AR"); dump("AI", AI, "AI"); dump("DG", DG, "DG")
            NB = 4
            wbuf = [TA("wbuf%d" % i, [128, 8, 512], BF16) for i in range(NB)]
            wb_i = [0]

            def load_block(name, kcs, c0, ncol):
                i = wb_i[0] % NB
                wb_i[0] += 1
                buf = wbuf[i]
                view = buf[:].rearrange("p a b -> p (a b)")[:, 0:kcs * ncol].rearrange("p (a b) -> p a b", b=ncol)
                p.dma("sync", [lambda e: e.dma_start(out=view, in_=S[name][:, :, c0:c0 + ncol].rearrange("k p n -> p k n"))],
                      reads=["prepA"], writes=["wbuf%d" % i])
                return view, "wbuf%d" % i

            x32_ = [TA("x32_%d" % i, [128, 8, NT], F32) for i in range(2)]
            xb = TA("xb", [128, 8, NT], BF16)
            us = TA("us", [128, 4, 16, 32], BF16)
            up = TA("up", [128, 4, 2, HS + 16], F32)
            XLs = TA("XLs", [128, 2, 16, 32], F32)
            XP = TA("XP", [128, 2, 16, 32], BF16)
            Z4 = TA("Z4", [128, 4, 32], F32)
            C1 = TA("C1", [128, 2, 16, 2], F32); C2 = TA("C2", [128, 2, 16, 2], F32)
            rt = [TA("rt%d" % i, [128, 2, 32], F32) for i in range(2)]
            bqs = lambda t_: t_[:].unsqueeze(2).to_broadcast([128, 16, 2])
            p.op("dve", lambda e: e.tensor_copy(out=C1[:, 0], in_=bqs(AR)), reads=["AR"], writes=["C12"])
            p.op("dve", lambda e: e.tensor_copy(out=C1[:, 1], in_=bqs(AR)), reads=["AR", "C12"], writes=["C12"])
            p.op("dve", lambda e: e.tensor_copy(out=C2[:, 1], in_=bqs(AI)), reads=["AI", "C12"], writes=["C12"])
            p.op("dve", lambda e: e.tensor_scalar(out=C2[:, 0], in0=C2[:, 1], scalar1=-1.0, scalar2=None, op0=ALU.mult), reads=["C12"], writes=["C12"])
            zb = TA("zb", [128, 4, NT], BF16)
            plb = zb
            mixb = TA("mixb", [128, 4, NT], BF16)
            merged = TA("merged", [128, 8, NT], BF16)
            mb = merged
            sA = TA("sA", [128, 2, HS + 16], F32); sB = TA("sB", [128, 2, HS + 16], F32)
            tmp = [TA("tmp%d" % i, [128, NT], F32) for i in range(3)]
            tmp_i = [0]

            def nt_():
                i = tmp_i[0] % 3
                tmp_i[0] += 1
                return tmp[i], "tmp%d" % i
            lnm, lnr = tmp[0], tmp[1]
            sqb = merged
            lg = TA("lg", [128, 4, 32], F32); mx8 = TA("mx8", [128, 4, 8], F32); nmx = TA("nmx", [128, 4], F32)
            msk = TA("msk", [128, 4, 32], F32); ssum = TA("ssum", [128, 4], F32); msk16 = TA("msk16", [128, 4, 32], BF16)
            pos = TA("pos", [128, 4, 32], F32); gq = TA("gq", [128, 4, 32], F32); lgw = TA("lgw", [128, 4, 32], F32)
            d8 = TA("d8", [128, 4, 8], F32); d4 = TA("d4", [128, 4, 4], F32)
            hT16 = [tmp[1], tmp[2]]

            ln_state = {}

            def layer_norm(src, skey, gname, bname, xbf, xbkey, part):
                if part == "b":
                    b1, k1, b2, k2 = ln_state["banks"]
                if part == "a":
                    p.op("dve", lambda e: e.tensor_copy(out=xbf[:], in_=src[:]), reads=[skey], writes=[xbkey])
                    b1, k1 = nb()

                    def mm1(e):
                        for kc in range(8):
                            ins = e.matmul(b1[:], lhsT=onesb[:], rhs=xbf[:, kc, :], start=(kc == 0), stop=(kc == 7))
                        return ins
                    p.op("pe", mm1, reads=[xbkey, "onesb"], writes=[k1])
                    p.op("act", lambda e: e.activation(out=sqb[:], in_=src[:], func=AF.Square), reads=[skey], writes=["merged"])
                    b2, k2 = nb()

                    def mm2(e):
                        for kc in range(8):
                            ins = e.matmul(b2[:], lhsT=onesb[:], rhs=sqb[:, kc, :], start=(kc == 0), stop=(kc == 7))
                        return ins
                    p.op("pe", mm2, reads=["merged", "onesb"], writes=[k2])
                    ln_state["banks"] = (b1, k1, b2, k2)
                    return
                p.op("act", lambda e: e.activation(out=lnm[:], in_=b1[:], func=AF.Copy, scale=1.0 / 1024), reads=[k1], writes=["tmp0"])
                p.op("dve", lambda e: e.tensor_mul(out=lnr[:], in0=lnm[:], in1=lnm[:]), reads=["tmp0"], writes=["tmp1"])
                p.op("dve", lambda e: e.scalar_tensor_tensor(out=lnr[:], in0=b2[:], scalar=1.0 / 1024, in1=lnr[:], op0=ALU.mult, op1=ALU.subtract), reads=[k2, "tmp1"], writes=["tmp1"])
                p.op("act", lambda e: e.activation(out=lnr[:], in_=lnr[:], func=AF.Sqrt, bias=1e-5, scale=1.0), reads=["tmp1"], writes=["tmp1"])
                p.op("dve", lambda e: e.reciprocal(out=lnr[:], in_=lnr[:]), reads=["tmp1"], writes=["tmp1"])
                bc8 = lambda t: t[:].unsqueeze(1).to_broadcast([128, 8, NT])
                p.op("dve", lambda e: e.tensor_tensor(out=src[:], in0=src[:], in1=bc8(lnm), op=ALU.subtract), reads=[skey, "tmp0"], writes=[skey])
                p.op("dve", lambda e: e.tensor_tensor(out=src[:], in0=src[:], in1=bc8(lnr), op=ALU.mult), reads=[skey, "tmp1"], writes=[skey])
                for kc in range(8):
                    p.op("act", lambda e, kc=kc: e.activation(out=src[:, kc, :], in_=src[:, kc, :], func=AF.Identity,
                                                              scale=cst[gname][:, kc:kc + 1], bias=cst[bname][:, kc:kc + 1]),
                         reads=[skey, "cst"], writes=[skey])
            print("phase A sbuf remaining", nc.sbuf_bytes_remaining)

            def front(t, part):
                tok0 = t * NT
                first = (t == 0)
                x32 = x32_[t % 2]
                kx = "x32_%d" % (t % 2)
                if part == "a":
                    p.dma("sync", [lambda e, h=h, tok0=tok0: e.dma_start(out=x32[:, 4 * h:4 * h + 4, :],
                                                                         in_=D["xT"][4 * h:4 * h + 4, :, tok0:tok0 + NT].rearrange("k p n -> p k n"))
                                   for h in range(2)], writes=[kx])
                    p.op("dve", lambda e: e.tensor_copy(out=xb[:, 0:4, :], in_=x32[:, 0:4, :]), reads=[kx], writes=["xb"])
                    p.op("act", lambda e: e.activation(out=xb[:, 4:8, :], in_=x32[:, 4:8, :], func=AF.Copy), reads=[kx, "xb"], writes=["xb"])
                    if first:
                        p.op("pool", lambda e: e.memset(up[:, :, :, 0:16], 0.0), writes=["up"])
                        p.op("pool", lambda e: e.memset(Z4[:], 0.0), writes=["Z4"])
                    return
                for cb in range(2):
                    wv, wk = load_block("w_in", 8, cb * 512, 512)
                    for mi in range(4):
                        mo = cb * 4 + mi
                        bk, bkey = nb()

                        def mm(e, wv=wv, mi=mi, bk=bk):
                            for kc in range(8):
                                ins = e.matmul(bk[:], lhsT=wv[:, kc, mi * 128:(mi + 1) * 128], rhs=xb[:, kc, :], start=(kc == 0), stop=(kc == 7))
                            return ins
                        p.op("pe", mm, reads=[wk, "xb"], writes=[bkey])
                        if mo < 4:
                            p.op("dve", lambda e, mo=mo, bk=bk: e.tensor_copy(out=us[:, mo].rearrange("p j c -> p c j"), in_=bk[:].rearrange("p (c j) -> p c j", j=16)), reads=[bkey], writes=["us"])
                        else:
                            p.op("act", lambda e, mo=mo, bk=bk: e.activation(out=up[:, mo - 4, :, 16:16 + HS], in_=bk[:].rearrange("p (s j) -> p s j", s=2), func=AF.Copy), reads=[bkey], writes=["up"])
                def mmx(e):
                    for q in range(16):
                        ch, a = q // 4, q % 4
                        for ri in range(2):
                            o = banks[6 + ri][:, q * 32:(q + 1) * 32]
                            for i in range(16):
                                ins = e.matmul(o, lhsT=WT[32 * a:32 * a + 32, ch, 15 - i, ri, :], rhs=us[32 * a:32 * a + 32, ch, i, :],
                                               start=(i == 0), stop=(i == 15), skip_group_check=True, tile_position=(32 * a, 0))
                    return ins
                p.op("pe", mmx, reads=["WT", "us"], writes=["pb6", "pb7"])
                p.op("dve", lambda e: e.tensor_copy(out=XLs[:, 0].rearrange("p cc x -> p x cc"), in_=banks[6][:].rearrange("p (x cc) -> p x cc", cc=16)), reads=["pb6"], writes=["XLs"])
                p.op("act", lambda e: e.activation(out=XLs[:, 1].rearrange("p cc x -> p x cc"), in_=banks[7][:].rearrange("p (x cc) -> p x cc", cc=16), func=AF.Copy), reads=["pb7", "XLs"], writes=["XLs"])
                dump("us", us, "us"); dump("up", up, "up"); dump("XLs", XLs, "XLs")

                W_ = HS + 16
                for g, w in enumerate((2, 4, 8, 16)):
                    p.op("pool", lambda e, g=g: e.tensor_tensor(out=sA[:, :, 1:W_], in0=up[:, g, :, 1:W_], in1=up[:, g, :, 0:W_ - 1], op=ALU.add), reads=["up", "sB"], writes=["sA"])
                    cur, curk, oth, othk = sA, "sA", sB, "sB"
                    sh = 1
                    while sh * 2 < w:
                        sh2 = sh * 2
                        lo = 2 * sh2 - 1
                        p.op("pool", lambda e, cur=cur, oth=oth, lo=lo, sh2=sh2: e.tensor_tensor(out=oth[:, :, lo:W_], in0=cur[:, :, lo:W_], in1=cur[:, :, lo - sh2:W_ - sh2], op=ALU.add),
                             reads=[curk], writes=[othk])
                        cur, curk, oth, othk = oth, othk, cur, curk
                        sh = sh2
                    plv = plb[:, g, :].rearrange("p (s j) -> p s j", s=2)
                    p.op("dve", lambda e, cur=cur, g=g, w=w, plv=plv: e.scalar_tensor_tensor(out=plv, in0=cur[:, :, 16:W_], scalar=1.0 / w, in1=up[:, g, :, 16:W_], op0=ALU.mult, op1=ALU.subtract),
                         reads=[curk, "up"], writes=["zb"])
                    if first:
                        p.op("dve", lambda e, cur=cur, g=g: e.tensor_tensor(out=cur[:, :, 16:32], in0=cur[:, :, 16:32],
                                                                             in1=cst["invc"][:, g, :].unsqueeze(1).to_broadcast([128, 2, 16]), op=ALU.mult), reads=[curk, "cst", "zb"], writes=[curk])
                        p.op("dve", lambda e, cur=cur, g=g, plv=plv: e.tensor_tensor(out=plv[:, :, 0:16], in0=cur[:, :, 16:32], in1=up[:, g, :, 16:32], op=ALU.subtract), reads=[curk, "up"], writes=["zb"])
                p.op("pool", lambda e: e.tensor_copy(out=up[:, :, :, 0:16], in_=up[:, :, :, HS:HS + 16]), reads=["up"], writes=["up"])

            def mid(t):
                tok0 = t * NT
                first = (t == 0)
                x32 = x32_[t % 2]
                kx = "x32_%d" % (t % 2)
                for g in range(4):
                    bk, bkey = nb()
                    p.op("pe", lambda e, g=g, bk=bk: e.matmul(bk[:], lhsT=wpgb[:, g, :], rhs=plb[:, g, :], start=True, stop=True), reads=["wpgb", "zb"], writes=[bkey])
                    p.op("act", lambda e, g=g, bk=bk: e.activation(out=mixb[:, g, :], in_=bk[:], func=AF.Copy, scale=cst["pscale"][:, g:g + 1]), reads=[bkey, "cst"], writes=["mixb"])
                wpv, wpk = load_block("w_pp", 4, 0, 1024)
                for mo in range(8):
                    bk, bkey = nb()

                    def mmp(e, mo=mo, bk=bk):
                        for kc in range(4):
                            ins = e.matmul(bk[:], lhsT=wpv[:, kc, mo * 128:(mo + 1) * 128], rhs=mixb[:, kc, :], start=(kc == 0), stop=(kc == 3))
                        return ins
                    if mo % 4 == 0:
                        wbv, wbk = load_block("w_in", 8, 2048 + (mo // 4) * 512, 512)
                    bb_, bbk = nb()

                    def mmb(e, mo=mo, bb_=bb_, wbv=wbv):
                        for kc in range(8):
                            ins = e.matmul(bb_[:], lhsT=wbv[:, kc, (mo % 4) * 128:(mo % 4 + 1) * 128], rhs=xb[:, kc, :], start=(kc == 0), stop=(kc == 7))
                        return ins
                    p.op("pe", mmp, reads=[wpk, "mixb"], writes=[bkey])
                    p.op("pe", mmb, reads=[wbk, "xb"], writes=[bbk])
                    t3, t3k = nt_()
                    p.op("act", lambda e, bb_=bb_, t3=t3: e.activation(out=t3[:], in_=bb_[:], func=AF.Sigmoid), reads=[bbk], writes=[t3k])
                    p.op("dve", lambda e, mo=mo, bk=bk, t3=t3: e.tensor_tensor(out=merged[:, mo, :], in0=bk[:], in1=t3[:], op=ALU.mult), reads=[bkey, t3k], writes=["merged"])
                c1v = C1[:].rearrange("p r q s -> p r (q s)"); c2v = C2[:].rearrange("p r q s -> p r (q s)")
                for cc in range(16):
                    p.op("pool", lambda e, cc=cc: e.tensor_copy(out=XP[:, :, :, cc::16], in_=Z4[:, 0:2, :].rearrange("p r (q s) -> p r q s", s=2)), reads=["Z4"], writes=["XP"])
                    p.op("pool", lambda e: e.tensor_tensor(out=rt[0][:], in0=c1v, in1=Z4[:, 0:2, :], op=ALU.mult), reads=["C12", "Z4"], writes=["rt0"])
                    p.op("pool", lambda e: e.tensor_tensor(out=rt[1][:], in0=c2v, in1=Z4[:, 1:3, :], op=ALU.mult), reads=["C12", "Z4"], writes=["rt1"])
                    p.op("pool", lambda e: e.tensor_tensor(out=rt[0][:], in0=rt[0][:], in1=rt[1][:], op=ALU.add), reads=["rt0", "rt1"], writes=["rt0"])
                    p.op("pool", lambda e, cc=cc: e.tensor_tensor(out=Z4[:].rearrange("p (a r) x -> p a r x", a=2),
                                                                   in0=rt[0][:].unsqueeze(1).to_broadcast([128, 2, 2, 32]),
                                                                   in1=XLs[:, :, cc, :].unsqueeze(1).to_broadcast([128, 2, 2, 32]), op=ALU.add),
                         reads=["rt0", "XLs", "XP"], writes=["Z4"])
                for ch in range(4):
                    bk, bkey = nb()

                    def mmy(e, ch=ch, bk=bk):
                        uf = us[:, ch].rearrange("p j c -> p (j c)")
                        e.matmul(bk[:], lhsT=KL[:, ch, 0, :], rhs=uf, start=True, stop=False, skip_group_check=True)
                        e.matmul(bk[:], lhsT=DG[:, ch, :], rhs=uf, start=False, stop=False, skip_group_check=True)
                        for kk in range(1, 16):
                            e.matmul(bk[:, kk * 32:512], lhsT=KL[:, ch, kk, :], rhs=uf[:, 0:(16 - kk) * 32], start=False, stop=False, skip_group_check=True)
                        for a in range(4):
                            q = 4 * ch + a
                            for j in range(16):
                                for ri in range(2):
                                    ins = e.matmul(bk[32 * a:32 * a + 32, j * 32:(j + 1) * 32], lhsT=GS[:, q, j, ri, :], rhs=XP[:, ri, q, :], start=False,
                                                   stop=(a == 3 and j == 15 and ri == 1), skip_group_check=True, tile_position=(0, 32 * a))
                        return ins
                    p.op("pe", mmy, reads=["KL", "DG", "GS", "us", "XP"], writes=[bkey])
                    p.op("act", lambda e, ch=ch, bk=bk: e.activation(out=zb[:, ch, :].rearrange("p (c j) -> p c j", j=16), in_=bk[:].rearrange("p (j c) -> p c j", c=32),
                                                                     func=AF.Gelu_apprx_tanh), reads=[bkey], writes=["zb"])
                dump("XP", XP, "XP"); dump("zb", zb, "zb")
                wvv, wvk = load_block("w_val", 4, 0, 1024)
                wgv, wgk = load_block("w_gg", 4, 0, 1024)
                for mo in range(8):
                    bv, bvk = nb(); bg, bgk = nb()

                    def mmv(e, mo=mo, bv=bv):
                        for kc in range(4):
                            ins = e.matmul(bv[:], lhsT=wvv[:, kc, mo * 128:(mo + 1) * 128], rhs=zb[:, kc, :], start=(kc == 0), stop=(kc == 3))
                        return ins

                    def mmg(e, mo=mo, bg=bg):
                        for kc in range(4):
                            ins = e.matmul(bg[:], lhsT=wgv[:, kc, mo * 128:(mo + 1) * 128], rhs=zb[:, kc, :], start=(kc == 0), stop=(kc == 3))
                        return ins
                    if mo % 4 == 0:
                        wav, wak = load_block("w_in", 8, 1024 + (mo // 4) * 512, 512)
                    ba, bak = nb()

                    def mma(e, mo=mo, ba=ba, wav=wav):
                        for kc in range(8):
                            ins = e.matmul(ba[:], lhsT=wav[:, kc, (mo % 4) * 128:(mo % 4 + 1) * 128], rhs=xb[:, kc, :], start=(kc == 0), stop=(kc == 7))
                        return ins
                    p.op("pe", mmv, reads=[wvk, "zb"], writes=[bvk])
                    p.op("pe", mmg, reads=[wgk, "zb"], writes=[bgk])
                    p.op("pe", mma, reads=[wak, "xb"], writes=[bak])
                    t1, t1k = nt_(); t2, t2k = nt_(); t3, t3k = nt_()
                    p.op("act", lambda e, bg=bg, t1=t1: e.activation(out=t1[:], in_=bg[:], func=AF.Sigmoid), reads=[bgk], writes=[t1k])
                    p.op("act", lambda e, ba=ba, t3=t3: e.activation(out=t3[:], in_=ba[:], func=AF.Sigmoid), reads=[bak], writes=[t3k])
                    p.op("dve", lambda e, bv=bv, t1=t1, t2=t2: e.tensor_tensor(out=t2[:], in0=bv[:], in1=t1[:], op=ALU.mult), reads=[bvk, t1k], writes=[t2k])
                    p.op("dve", lambda e, t2=t2, t3=t3: e.tensor_tensor(out=t2[:], in0=t2[:], in1=t3[:], op=ALU.mult), reads=[t2k, t3k], writes=[t2k])
                    p.op("pool", lambda e, mo=mo, t2=t2: e.tensor_tensor(out=merged[:, mo, :], in0=merged[:, mo, :], in1=t2[:], op=ALU.add), reads=[t2k, "merged"], writes=["merged"])
                dump("zb", plb, "zb"); dump("mixb", mixb, "mixb"); dump("merged", merged, "merged")
                for cb in range(2):
                    wv, wk = load_block("w_out", 8, cb * 512, 512)
                    for mi in range(4):
                        mo = cb * 4 + mi
                        bk, bkey = nb()

                        def mmo(e, wv=wv, mi=mi, bk=bk):
                            for kc in range(8):
                                ins = e.matmul(bk[:], lhsT=wv[:, kc, mi * 128:(mi + 1) * 128], rhs=merged[:, kc, :], start=(kc == 0), stop=(kc == 7))
                            return ins
                        p.op("pe", mmo, reads=[wk, "merged"], writes=[bkey])
                        p.op("dve", lambda e, mo=mo, bk=bk: e.scalar_tensor_tensor(out=x32[:, mo, :], in0=x32[:, mo, :], scalar=ALPHA, in1=bk[:], op0=ALU.mult, op1=ALU.add),
                             reads=[bkey, kx], writes=[kx])


            def tail(t, part):
                tok0 = t * NT
                first = (t == 0)
                x32 = x32_[t % 2]
                kx = "x32_%d" % (t % 2)
                if part == "a":
                    dump("pre1", x32, kx)
                    layer_norm(x32, kx, "ln1g", "ln1b", merged, "merged", "a")
                    return
                if part == "b":
                    layer_norm(x32, kx, "ln1g", "ln1b", merged, "merged", "b")
                    p.dma("act", [lambda e, h=h, tok0=tok0: e.dma_start(out=h1T[4 * h:4 * h + 4, :, tok0:tok0 + NT].rearrange("k p n -> p k n"), in_=x32[:, 4 * h:4 * h + 4, :])
                                  for h in range(2)], reads=[kx], writes=["h1T"])
                    return
                bk, bkey = nb()

                def mmr(e, bk=bk):
                    for blk in range(4):
                        for kc in range(8):
                            ins = e.matmul(bk[:, blk * 32:(blk + 1) * 32], lhsT=x32[:, kc, blk * 128:(blk + 1) * 128], rhs=cst["w_r"][:, kc, :],
                                           start=(kc == 0), stop=(kc == 7), skip_group_check=True)
                    return ins
                p.op("pe", mmr, reads=[kx, "cst"], writes=[bkey])
                v3 = lambda ap: ap.rearrange("p (b e) -> p b e", e=32)
                p.op("dve", lambda e, bk=bk: e.tensor_tensor(out=lg[:], in0=v3(bk[:, 0:128]),
                                                             in1=cst["b_r"][:].unsqueeze(1).to_broadcast([128, 4, 32]), op=ALU.add), reads=[bkey, "cst"], writes=["lg"])
                for blk in range(4):
                    hT, hk = hT16[blk % 2][:].bitcast(BF16), "tmp%d" % (1 + blk % 2)
                    for half in range(2):
                        bk, bkey = nb()

                        def mmT(e, bk=bk, blk=blk, half=half):
                            for j in range(4):
                                ins = e.transpose(bk[:, j * 128:(j + 1) * 128], x32[:, half * 4 + j, blk * 128:(blk + 1) * 128], cst["ident"][:])
                            return ins
                        p.op("pe", mmT, reads=[kx, "cst"], writes=[bkey])
                        p.op("act", lambda e, bk=bk, hT=hT, half=half: e.activation(out=hT[:, half * 512:(half + 1) * 512], in_=bk[:], func=AF.Copy), reads=[bkey], writes=[hk])
                    p.dma("act", [lambda e, hT=hT, r0=tok0 + blk * 128: e.dma_start(out=h1tm16[r0:r0 + 128, :], in_=hT)],
                          reads=[hk], writes=["h1tm_%d" % (4 * t + blk)], key="h1tm16")

                for blk in range(4):
                    p.op("dve", lambda e, blk=blk: e.max(out=mx8[:, blk, :], in_=lg[:, blk, :]), reads=["lg"], writes=["mx8"])
                for blk in range(4):
                    p.op("dve", lambda e, blk=blk: e.tensor_scalar(out=msk[:, blk, :], in0=lg[:, blk, :], scalar1=mx8[:, blk, 3:4], scalar2=None, op0=ALU.is_ge), reads=["lg", "mx8"], writes=["msk"])
                p.op("dve", lambda e: e.tensor_scalar(out=nmx[:], in0=mx8[:, :, 0], scalar1=-1.0, scalar2=None, op0=ALU.mult), reads=["mx8"], writes=["nmx"])
                for blk in range(4):
                    p.op("act", lambda e, blk=blk: e.activation(out=lg[:, blk, :], in_=lg[:, blk, :], func=AF.Exp, bias=nmx[:, blk:blk + 1], scale=1.0), reads=["lg", "nmx", "msk"], writes=["lg"])
                p.op("dve", lambda e: e.tensor_mul(out=lg[:], in0=lg[:], in1=msk[:]), reads=["lg", "msk"], writes=["lg"])
                p.op("dve", lambda e: e.reduce_sum(out=ssum[:], in_=lg[:], axis=AX.X), reads=["lg"], writes=["ssum"])
                p.op("dve", lambda e: e.reciprocal(out=ssum[:], in_=ssum[:]), reads=["ssum"], writes=["ssum"])
                p.op("dve", lambda e: e.tensor_tensor(out=lg[:], in0=lg[:], in1=ssum[:].unsqueeze(2).to_broadcast([128, 4, 32]), op=ALU.mult), reads=["lg", "ssum"], writes=["lg"])
                p.op("dve", lambda e: e.tensor_copy(out=lgw[:], in_=lg[:]), reads=["lg"], writes=["lgw"])
                deferred.append(lambda tok0=tok0, t=t: p.dma("pool", [lambda e, tok0=tok0: e.dma_start(out=wtab[tok0:tok0 + NT, :].rearrange("(b p) e -> p b e", p=128), in_=lgw[:])],
                                                             reads=["lgw"], writes=["wtab_%d" % t], key="wtab"))
                p.op("dve", lambda e: e.tensor_copy(out=msk16[:], in_=msk[:]), reads=["msk"], writes=["msk16"])
                bk, bkey = nb()

                def mmc(e, bk=bk):
                    for b in range(4):
                        ins = e.matmul(bk[:, b * 32:(b + 1) * 32], lhsT=TRI[:], rhs=msk16[:, b, :], start=True, stop=True, skip_group_check=True)
                    for b in range(1, 5):
                        for b2 in range(b):
                            ins = e.matmul(bk[:, 128 + b * 32:128 + (b + 1) * 32], lhsT=onesb[:], rhs=msk16[:, b2, :], start=(b2 == 0), stop=(b2 == b - 1), skip_group_check=True)
                    return ins
                p.op("pe", mmc, reads=["msk16", "TRI", "onesb"], writes=[bkey])
                p.op("dve", lambda e, bk=bk: e.tensor_tensor(out=pos[:], in0=v3(bk[:, 0:128]), in1=CNT[:].unsqueeze(1).to_broadcast([128, 4, 32]), op=ALU.add), reads=[bkey, "CNT"], writes=["pos"])
                p.op("dve", lambda e, bk=bk: e.tensor_tensor(out=pos[:, 1:4, :], in0=pos[:, 1:4, :], in1=v3(bk[:, 160:256]), op=ALU.add), reads=[bkey, "pos"], writes=["pos"])
                p.op("dve", lambda e, bk=bk: e.tensor_tensor(out=CNT[:], in0=CNT[:], in1=bk[:, 256:288], op=ALU.add), reads=[bkey, "CNT", "pos"], writes=["CNT"])
                p.op("dve", lambda e: e.tensor_single_scalar(out=gq[:], in_=pos[:], scalar=float(cap), op=ALU.is_ge), reads=["pos"], writes=["gq"])
                p.op("dve", lambda e: e.tensor_scalar(out=gq[:], in0=gq[:], scalar1=1.0e6, scalar2=None, op0=ALU.mult), reads=["gq"], writes=["gq"])
                p.op("dve", lambda e: e.tensor_add(out=pos[:], in0=pos[:], in1=gq[:]), reads=["pos", "gq"], writes=["pos"])
                p.op("dve", lambda e: e.tensor_tensor(out=pos[:], in0=pos[:], in1=cst["eoff1"][:].unsqueeze(1).to_broadcast([128, 4, 32]), op=ALU.add), reads=["pos", "cst"], writes=["pos"])
                p.op("dve", lambda e: e.tensor_mul(out=pos[:], in0=pos[:], in1=msk[:]), reads=["pos", "msk"], writes=["pos"])
                p.op("dve", lambda e: e.tensor_scalar(out=pos[:], in0=pos[:], scalar1=-1.0, scalar2=None, op0=ALU.add), reads=["pos"], writes=["pos"])
                for blk in range(4):
                    p.op("dve", lambda e, blk=blk: e.max(out=d8[:, blk, :], in_=pos[:, blk, :]), reads=["pos"], writes=["d8"])
                p.op("dve", lambda e: e.tensor_scalar(out=d4[:], in0=d8[:, :, 0:4], scalar1=float(NSLOT), scalar2=None, op0=ALU.min), reads=["d8"], writes=["d4"])
                p.op("dve", lambda e, t=t: e.tensor_copy(out=DEST[:, 4 * t:4 * t + 4, :], in_=d4[:]), reads=["d4"], writes=["DEST%d" % t])
                for blk in range(4):
                    gb = 4 * t + blk
                    for kk in range(4):
                        deferred.append(lambda gb=gb, kk=kk, t=t: p.dma("pool", [lambda e, gb=gb, kk=kk: e.indirect_dma_start(
                            out=toklist[:, :], out_offset=bass.IndirectOffsetOnAxis(ap=DEST[:, gb, kk:kk + 1], axis=0),
                            in_=TOKID[:, gb:gb + 1], in_offset=None, bounds_check=bcr(e, NSLOT - 1), oob_is_err=False)],
                            reads=["DEST%d" % t, "TOKID", "init"], writes=["tl_%d_%d" % (gb, kk)], key="toklist"))
            deferred = []

            def flush_scatters():
                for f_ in deferred:
                    f_()
                del deferred[:]
            front(0, "a")
            front(0, "b")
            for t in range(ntiles):
                mid(t)
                flush_scatters()
                tail(t, "a")
                if t + 1 < ntiles:
                    front(t + 1, "a")
                tail(t, "b")
                if t + 1 < ntiles:
                    front(t + 1, "b")
                tail(t, "c")
            flush_scatters()
            p.seal("toklist", ["toklist"])
            p.seal("h1tm16", ["h1tm16"])
            p.seal("wtab", ["wtab"])

        p.barrier()
        with ExitStack() as esB:
            TB = lambda n, s, d: T(n, s, d, esB)
            NEB = 4
            ebuf = [TB("ebuf%d" % i, [128, 8, 1024], BF16) for i in range(NEB)]
            eb_i = [0]

            def load_expert(name, ex):
                i = eb_i[0] % NEB
                eb_i[0] += 1
                buf = ebuf[i]
                p.dma("pool", [lambda e, h=h: e.dma_start(out=buf[:, 2 * h:2 * h + 2, :], in_=D[name][ex, 2 * h:2 * h + 2].rearrange("k p n -> p k n")) for h in range(4)],
                      writes=["ebuf%d" % i])
                return buf, "ebuf%d" % i
            for name in ["b_eg", "b_eu"]:
                cst[name] = TB("c_" + name, [128, 32, 8], F32)
            p.dma("sync", [lambda e, name=name: e.dma_start(out=cst[name][:], in_=D[name]) for name in ["b_eg", "b_eu"]], writes=["cstB"], key="cstB")
            p.seal("cstB", ["cstB"])
            subs = [(s0, min(512, cap - s0)) for s0 in range(0, cap, 512)]
            Xfm = TB("Xfm", [128, 8, cap], BF16)
            Afm = TB("Afm", [128, 8, cap], BF16)
            NXB = 12
            xtm = [TB("xtm%d" % i, [128, 1024], BF16) for i in range(NXB)]
            tlb = [TB("tlb%d" % i, [128, NBLK], I32) for i in range(2)]
            wsl = [[TB("wsl%d_%d" % (i, b), [128, 32], F32) for b in range(NBLK)] for i in range(2)]
            bdr = [TB("bdr%d" % i, [1, 1024], F32) for i in range(2)]
            ones32 = TB("ones16", [1, 128], BF16)
            p.op("dve", lambda e: e.memset(ones32[:], 1.0), writes=["ones32"])
            bdb = [TB("bdb%d" % i, [1, 1024], BF16) for i in range(2)]
            ysb = [TB("ysb%d" % i, [128, 1024], F32) for i in range(3)]
            tmpB = [TB("tb%d" % i, [128, NT], F32) for i in range(6)]
            tb_i = [0]; xb_i = [0]; ys_i = [0]

            def ntb():
                i = tb_i[0] % 6
                tb_i[0] += 1
                return tmpB[i], "tb%d" % i

            def stage_in(ex):
                tl, tlk = tlb[ex % 2], "tlb%d" % (ex % 2)
                ws, wsk = wsl[ex % 2], "wsl%d" % (ex % 2)
                p.dma("sync", [lambda e: e.dma_start(out=tl[:], in_=toklist[ex * cap:(ex + 1) * cap, :].rearrange("(p b) o -> p (b o)", b=NBLK)),
                               lambda e: e.dma_start(out=bdr[ex % 2][:], in_=D["b_down"][ex:ex + 1, :])],
                      reads=["toklist"], writes=[tlk, "bdr%d" % (ex % 2)])
                p.op("dve", lambda e: e.tensor_copy(out=bdb[ex % 2][:], in_=bdr[ex % 2][:]), reads=["bdr%d" % (ex % 2)], writes=["bdb%d" % (ex % 2)])
                for b in range(NBLK):
                    i = xb_i[0] % NXB
                    xb_i[0] += 1
                    xt, xk = xtm[i], "xtm%d" % i
                    p.dma("pool", [lambda e, b=b, xt=xt: e.indirect_dma_start(out=xt[:, :], out_offset=None, in_=h1tm16[:, :],
                                                                               in_offset=bass.IndirectOffsetOnAxis(ap=tl[:, b:b + 1], axis=0),
                                                                               bounds_check=bcr(e, ntok), oob_is_err=False)],
                          reads=[tlk, "h1tm16", "init"], writes=[xk])
                    p.dma("pool", [lambda e, b=b: e.indirect_dma_start(out=ws[b][:, :], out_offset=None, in_=wtab[:, :],
                                                                       in_offset=bass.IndirectOffsetOnAxis(ap=tl[:, b:b + 1], axis=0),
                                                                       bounds_check=bcr(e, ntok), oob_is_err=False)],
                          reads=[tlk, "wtab", "init"], writes=[wsk + "_%d" % b], key=wsk)
                    bk, bkey = nb(8)
                    pbv = bk[:].bitcast(BF16).rearrange("p (k s) -> p k s", s=128)

                    def mmt(e, xt=xt, pbv=pbv):
                        for kc in range(8):
                            ins = e.transpose(pbv[:, kc, :], xt[:, kc * 128:(kc + 1) * 128], identb[:])
                        return ins
                    p.op("pe", mmt, reads=[xk, "identb"], writes=[bkey])
                    if b % 2 == 0:
                        p.op("dve", lambda e, b=b, pbv=pbv: e.tensor_copy(out=Xfm[:, :, b * 128:(b + 1) * 128], in_=pbv), reads=[bkey], writes=["Xfm"])
                    else:
                        p.op("act", lambda e, b=b, pbv=pbv: e.activation(out=Xfm[:, :, b * 128:(b + 1) * 128], in_=pbv, func=AF.Copy), reads=[bkey], writes=["Xfm"])
                p.seal(wsk, [wsk])

            wl = {}

            def prefetch(name, ex):
                if ex < n_experts:
                    wl[(name, ex)] = load_expert(name, ex)

            def stage_gu(ex):
                wgb, wgk = wl[("w_eg", ex)]
                wub, wuk = wl[("w_eu", ex)]
                for (s0, n) in subs:
                    for mo in range(8):
                        bg, bgk = nb(8); bu, buk = nb(8)

                        def mmg(e, mo=mo, bg=bg, wgb=wgb, s0=s0, n=n):
                            for kc in range(8):
                                ins = e.matmul(bg[:, 0:n], lhsT=wgb[:, kc, mo * 128:(mo + 1) * 128], rhs=Xfm[:, kc, s0:s0 + n], start=(kc == 0), stop=(kc == 7))
                            return ins

                        def mmu(e, mo=mo, bu=bu, wub=wub, s0=s0, n=n):
                            for kc in range(8):
                                ins = e.matmul(bu[:, 0:n], lhsT=wub[:, kc, mo * 128:(mo + 1) * 128], rhs=Xfm[:, kc, s0:s0 + n], start=(kc == 0), stop=(kc == 7))
                            return ins
                        p.op("pe", mmg, reads=[wgk, "Xfm"], writes=[bgk])
                        p.op("pe", mmu, reads=[wuk, "Xfm"], writes=[buk])
                        g_, gk = ntb(); sg, sgk = ntb(); u_, uk = ntb()
                        p.op("dve", lambda e, mo=mo, bg=bg, g_=g_, n=n: e.tensor_scalar(out=g_[:, 0:n], in0=bg[:, 0:n], scalar1=cst["b_eg"][:, ex, mo:mo + 1], scalar2=7.0, op0=ALU.add, op1=ALU.min),
                             reads=[bgk, "cstB"], writes=[gk])
                        p.op("act", lambda e, g_=g_, sg=sg, n=n: e.activation(out=sg[:, 0:n], in_=g_[:, 0:n], func=AF.Sigmoid, scale=1.702), reads=[gk], writes=[sgk])
                        p.op("act", lambda e, mo=mo, bu=bu, u_=u_, n=n: e.activation(out=u_[:, 0:n], in_=bu[:, 0:n], func=AF.Identity, bias=cst["b_eu"][:, ex, mo:mo + 1], scale=1.0),
                             reads=[buk, "cstB"], writes=[uk])
                        p.op("dve", lambda e, u_=u_, n=n: e.tensor_scalar(out=u_[:, 0:n], in0=u_[:, 0:n], scalar1=7.0, scalar2=-7.0, op0=ALU.min, op1=ALU.max), reads=[uk], writes=[uk])
                        p.op("dve", lambda e, u_=u_, g_=g_, n=n: e.scalar_tensor_tensor(out=u_[:, 0:n], in0=u_[:, 0:n], scalar=1.0, in1=g_[:, 0:n], op0=ALU.add, op1=ALU.mult), reads=[uk, gk], writes=[uk])
                        p.op("dve", lambda e, mo=mo, u_=u_, sg=sg, s0=s0, n=n: e.tensor_tensor(out=Afm[:, mo, s0:s0 + n], in0=u_[:, 0:n], in1=sg[:, 0:n], op=ALU.mult), reads=[uk, sgk], writes=["Afm"])

            def stage_down(ex):
                wdb, wdk = wl[("w_ed", ex)]
                ws, wsk = wsl[ex % 2], "wsl%d" % (ex % 2)
                bd_, bdk_ = bdb[ex % 2], "bdb%d" % (ex % 2)
                for b in range(NBLK):
                    i = ys_i[0] % 3
                    ys_i[0] += 1
                    yt, yk = ysb[i], "ysb%d" % i
                    for half in range(2):
                        bk, bkey = nb(8)

                        def mmd(e, b=b, half=half, bk=bk):
                            for kc in range(8):
                                e.matmul(bk[:], lhsT=Afm[:, kc, b * 128:(b + 1) * 128], rhs=wdb[:, kc, half * 512:(half + 1) * 512], start=(kc == 0), stop=False, skip_group_check=True)
                            return e.matmul(bk[:], lhsT=ones32[:], rhs=bd_[:, half * 512:(half + 1) * 512], start=False, stop=True, skip_group_check=True)
                        p.op("pe", mmd, reads=[wdk, "Afm", "ones32", bdk_], writes=[bkey])
                        p.op("act", lambda e, b=b, half=half, bk=bk, yt=yt: e.activation(out=yt[:, half * 512:(half + 1) * 512], in_=bk[:], func=AF.Copy, scale=ws[b][:, ex:ex + 1]),
                             reads=[bkey, wsk], writes=[yk])
                    p.dma("sync", [lambda e, yt=yt, b=b: e.dma_start(out=ysd[ex * cap:(ex + 1) * cap, :].rearrange("(p b) d -> p b d", b=NBLK)[:, b, :], in_=yt[:])],
                          reads=[yk], writes=["ys_%d_%d" % (ex, b)], key="ysd")

            prefetch("w_eg", 0)
            prefetch("w_eu", 0)
            stage_in(0)
            for ex in range(n_experts):
                prefetch("w_ed", ex)
                prefetch("w_eg", ex + 1)
                stage_gu(ex)
                if ex + 1 < n_experts:
                    stage_in(ex + 1)
                prefetch("w_eu", ex + 1)
                stage_down(ex)
            p.seal("ysd", ["ysd"])

        p.barrier()
        with ExitStack() as esC:
            TB = lambda n, s, d: T(n, s, d, esC)
            wplg = TB("wplg", [128, 8, 1024], BF16)
            wplp = TB("wplp", [128, 2, 1024], BF16)
            p.dma("sync", [lambda e: e.dma_start(out=wplg[:], in_=S["w_plg"].rearrange("k p n -> p k n")),
                           lambda e: e.dma_start(out=wplp[:], in_=S["w_plp"].rearrange("k p n -> p k n"))], reads=["prepA"], writes=["wpl"], key="wpl")
            p.seal("wpl", ["wpl"])
            h32_ = [TB("h32_%d" % i, [128, 8, NT], F32) for i in range(2)]
            hb_ = [TB("hb_%d" % i, [128, 8, NT], BF16) for i in range(2)]
            acc_ = [TB("acc_%d" % i, [128, 8, NT], F32) for i in range(2)]
            p32_ = [TB("p32_%d" % i, [128, 2, NT], F32) for i in range(2)]
            pb16_ = [TB("pb16_%d" % i, [128, 2, NT], BF16) for i in range(2)]
            ybuf = [TB("yb%d" % i, [128, 1024], F32) for i in range(12)]
            tmpB = [TB("tc%d" % i, [128, NT], F32) for i in range(6)]
            tb_i = [0]

            def ntb():
                i = tb_i[0] % 6
                tb_i[0] += 1
                return tmpB[i], "tc%d" % i
            lnm = TB("lnmB", [128, NT], F32); lnr = TB("lnrB", [128, NT], F32)
            sqb = TB("sqbB", [128, 8, NT], BF16)

            def layer_norm2(src, skey, gname, bname, xbf, xbkey):
                p.op("dve", lambda e: e.tensor_copy(out=xbf[:], in_=src[:]), reads=[skey], writes=[xbkey])
                p.op("act", lambda e: e.activation(out=sqb[:], in_=src[:], func=AF.Square), reads=[skey], writes=["sqbB"])
                b1, k1 = nb(8); b2, k2 = nb(8)

                def mm1(e):
                    for kc in range(8):
                        ins = e.matmul(b1[:], lhsT=onesb[:], rhs=xbf[:, kc, :], start=(kc == 0), stop=(kc == 7))
                    return ins

                def mm2(e):
                    for kc in range(8):
                        ins = e.matmul(b2[:], lhsT=onesb[:], rhs=sqb[:, kc, :], start=(kc == 0), stop=(kc == 7))
                    return ins
                p.op("pe", mm1, reads=[xbkey, "onesb"], writes=[k1])
                p.op("pe", mm2, reads=["sqbB", "onesb"], writes=[k2])
                p.op("act", lambda e: e.activation(out=lnm[:], in_=b1[:], func=AF.Copy, scale=1.0 / 1024), reads=[k1], writes=["lnmB"])
                p.op("dve", lambda e: e.tensor_mul(out=lnr[:], in0=lnm[:], in1=lnm[:]), reads=["lnmB"], writes=["lnrB"])
                p.op("dve", lambda e: e.scalar_tensor_tensor(out=lnr[:], in0=b2[:], scalar=1.0 / 1024, in1=lnr[:], op0=ALU.mult, op1=ALU.subtract), reads=[k2, "lnrB"], writes=["lnrB"])
                p.op("act", lambda e: e.activation(out=lnr[:], in_=lnr[:], func=AF.Sqrt, bias=1e-5, scale=1.0), reads=["lnrB"], writes=["lnrB"])
                p.op("dve", lambda e: e.reciprocal(out=lnr[:], in_=lnr[:]), reads=["lnrB"], writes=["lnrB"])
                bc8 = lambda t: t[:].unsqueeze(1).to_broadcast([128, 8, NT])
                p.op("dve", lambda e: e.tensor_tensor(out=src[:], in0=src[:], in1=bc8(lnm), op=ALU.subtract), reads=[skey, "lnmB"], writes=[skey])
                p.op("dve", lambda e: e.tensor_tensor(out=src[:], in0=src[:], in1=bc8(lnr), op=ALU.mult), reads=[skey, "lnrB"], writes=[skey])
                for kc in range(8):
                    p.op("act", lambda e, kc=kc: e.activation(out=src[:, kc, :], in_=src[:, kc, :], func=AF.Identity,
                                                              scale=cst[gname][:, kc:kc + 1], bias=cst[bname][:, kc:kc + 1]),
                         reads=[skey, "cst"], writes=[skey])

            ysum = {}

            def gather_block(t, blk):
                gb = 4 * t + blk
                ys_ = []
                for kk in range(4):
                    i = (gb * 4 + kk) % 12
                    yb, ybk = ybuf[i], "yb%d" % i
                    p.dma("pool", [lambda e, gb=gb, kk=kk, yb=yb: e.indirect_dma_start(out=yb[:, :], out_offset=None, in_=ysd[:, :],
                                                                                     in_offset=bass.IndirectOffsetOnAxis(ap=DEST[:, gb, kk:kk + 1], axis=0),
                                                                                     bounds_check=bcr(e, NSLOT), oob_is_err=False)],
                          reads=["ysd", "init"], writes=[ybk])
                    ys_.append((yb, ybk))
                ysum[(t, blk)] = ys_

            def combine_a(t):
                tok0 = t * NT
                h32, hb, acc, p32, pb16 = h32_[t % 2], hb_[t % 2], acc_[t % 2], p32_[t % 2], pb16_[t % 2]
                kh32, khb, kacc, kp32, kpb16 = ["%s_%d" % (n_, t % 2) for n_ in ("h32", "hb", "acc", "p32", "pb16")]
                p.dma("sync", [lambda e, h=h, tok0=tok0: e.dma_start(out=h32[:, 4 * h:4 * h + 4, :], in_=h1T[4 * h:4 * h + 4, :, tok0:tok0 + NT].rearrange("k p n -> p k n"))
                               for h in range(2)], reads=["h1T"], writes=[kh32])
                p.dma("sync", [lambda e, tok0=tok0: e.dma_start(out=p32[:], in_=D["pT"][:, :, tok0:tok0 + NT].rearrange("k p n -> p k n"))], writes=[kp32])
                for blk in range(3):
                    gather_block(t, blk)

            def combine_b(t):
                tok0 = t * NT
                h32, hb, acc, p32, pb16 = h32_[t % 2], hb_[t % 2], acc_[t % 2], p32_[t % 2], pb16_[t % 2]
                kh32, khb, kacc, kp32, kpb16 = ["%s_%d" % (n_, t % 2) for n_ in ("h32", "hb", "acc", "p32", "pb16")]
                p.op("dve", lambda e: e.tensor_copy(out=hb[:], in_=h32[:]), reads=[kh32], writes=[khb])
                p.op("act", lambda e: e.activation(out=pb16[:], in_=p32[:], func=AF.Copy), reads=[kp32], writes=[kpb16])
                for blk in range(4):
                    ys_ = ysum[(t, blk)]
                    for half in range(2):
                        bk, bkey = nb(8)

                        def mmT2(e, bk=bk, ys_=ys_, half=half):
                            for j in range(4):
                                kc = half * 4 + j
                                for kk in range(4):
                                    ins = e.matmul(bk[:, j * 128:(j + 1) * 128], lhsT=ys_[kk][0][:, kc * 128:(kc + 1) * 128], rhs=cst["ident"][:],
                                                   start=(kk == 0), stop=(kk == 3), skip_group_check=True)
                            return ins
                        p.op("pe", mmT2, reads=[k_ for _, k_ in ys_] + ["cst"], writes=[bkey])
                        p.op("act", lambda e, bk=bk, half=half, blk=blk: e.activation(out=acc[:, 4 * half:4 * half + 4, blk * 128:(blk + 1) * 128],
                                                                                        in_=bk[:].rearrange("p (j s) -> p j s", s=128), func=AF.Copy), reads=[bkey], writes=[kacc])
                    if blk == 0:
                        gather_block(t, 3)

            def ple_ln(t):
                tok0 = t * NT
                h32, hb, acc, p32, pb16 = h32_[t % 2], hb_[t % 2], acc_[t % 2], p32_[t % 2], pb16_[t % 2]
                kh32, khb, kacc, kp32, kpb16 = ["%s_%d" % (n_, t % 2) for n_ in ("h32", "hb", "acc", "p32", "pb16")]
                for mo in range(8):
                    bg, bgk = nb(8); bp, bpk = nb(8)

                    def mmpg(e, mo=mo, bg=bg):
                        for kc in range(8):
                            ins = e.matmul(bg[:], lhsT=wplg[:, kc, mo * 128:(mo + 1) * 128], rhs=hb[:, kc, :], start=(kc == 0), stop=(kc == 7))
                        return ins

                    def mmpp(e, mo=mo, bp=bp):
                        for kc in range(2):
                            ins = e.matmul(bp[:], lhsT=wplp[:, kc, mo * 128:(mo + 1) * 128], rhs=pb16[:, kc, :], start=(kc == 0), stop=(kc == 1))
                        return ins
                    p.op("pe", mmpg, reads=["wpl", khb], writes=[bgk])
                    p.op("pe", mmpp, reads=["wpl", kpb16], writes=[bpk])
                    sg, sgk = ntb(); t1, t1k = ntb()
                    p.op("act", lambda e, bg=bg, sg=sg: e.activation(out=sg[:], in_=bg[:], func=AF.Sigmoid), reads=[bgk], writes=[sgk])
                    p.op("dve", lambda e, bp=bp, sg=sg, t1=t1: e.tensor_tensor(out=t1[:], in0=bp[:], in1=sg[:], op=ALU.mult), reads=[bpk, sgk], writes=[t1k])
                    p.op("dve", lambda e, mo=mo, t1=t1: e.tensor_tensor(out=t1[:], in0=acc[:, mo, :], in1=t1[:], op=ALU.add), reads=[t1k, kacc], writes=[t1k])
                    p.op("dve", lambda e, mo=mo, t1=t1: e.scalar_tensor_tensor(out=h32[:, mo, :], in0=h32[:, mo, :], scalar=ALPHA, in1=t1[:], op0=ALU.mult, op1=ALU.add),
                         reads=[t1k, kh32], writes=[kh32])
                layer_norm2(h32, kh32, "ln2g", "ln2b", hb, khb)
                p.dma("sync", [lambda e, h=h, tok0=tok0: e.dma_start(out=outT[4 * h:4 * h + 4, :, tok0:tok0 + NT].rearrange("k p n -> p k n"), in_=h32[:, 4 * h:4 * h + 4, :])
                               for h in range(2)], reads=[kh32], writes=["outT"])
            combine_a(0)
            combine_b(0)
            for t in range(ntiles):
                if t + 1 < ntiles:
                    combine_a(t + 1)
                ple_ln(t)
                if t + 1 < ntiles:
                    combine_b(t + 1)
            p.final_wait("sync", ["outT", "h1T"] + ["dbg_" + n for n in dbg_outs])
            p.final_wait("pool", ["prepA"])

        p.emit(block)
    return nc


def shared_layout(I, cap=1536):
    f = lambda a: np.ascontiguousarray(a, dtype=np.float32)
    out = {}
    out["w_in"] = f(I["w_in"][0].reshape(8, 128, 3072))
    out["w_val"] = f(I["w_glu_val"][0].reshape(4, 128, 1024))
    out["w_gg"] = f(I["w_glu_gate"][0].reshape(4, 128, 1024))
    out["w_pp"] = f(I["w_pool_proj"][0].reshape(4, 128, 1024))
    out["w_out"] = f(I["w_out"][0].reshape(8, 128, 1024))
    out["w_plg"] = f(I["w_ple_gate"][0].reshape(8, 128, 1024))
    out["w_plp"] = f(I["w_ple_proj"][0].reshape(2, 128, 1024))
    out["w_pg"] = f(I["w_pool_group"][0].transpose(1, 0, 2))
    out["pscale"] = f(I["pool_scale"][0].reshape(4, 128).T)
    out["ln1g"] = f(I["ln1_g"][0].reshape(8, 128).T)
    out["ln1b"] = f(I["ln1_b"][0].reshape(8, 128).T)
    out["ln2g"] = f(I["ln2_g"][0].reshape(8, 128).T)
    out["ln2b"] = f(I["ln2_b"][0].reshape(8, 128).T)
    out["w_r"] = f(I["w_router"][0].reshape(8, 128, 32).transpose(1, 0, 2))
    out["b_r"] = f(np.broadcast_to(I["b_router"][0][None, :], (128, 32)))
    out["w_eg"] = f(I["w_gate"][0].reshape(32, 8, 128, 1024))
    out["w_eu"] = f(I["w_up"][0].reshape(32, 8, 128, 1024))
    out["w_ed"] = f(I["w_down"][0].reshape(32, 8, 128, 1024))
    for n, s in [("b_eg", "b_gate"), ("b_eu", "b_up")]:
        out[n] = f(I[s][0].reshape(32, 8, 128).transpose(2, 0, 1))
    out["b_down"] = f(I["b_down"][0])
    lr, li, ls = I["ssm_lambda_re"][0], I["ssm_lambda_im"][0], I["ssm_log_step"][0]
    br, bi = I["ssm_b_re"][0], I["ssm_b_im"][0]
    cr, ci = I["ssm_c_re"][0], I["ssm_c_im"][0]
    dd = I["ssm_d"][0]
    g_S = (2 * np.arange(16)[None, :] + np.arange(2)[:, None])
    lrS = lr[g_S]
    out["lrS"] = f(lrS.transpose(0, 2, 1).reshape(128, 16))
    out["liS"] = f(li[g_S].transpose(0, 2, 1).reshape(128, 16))
    out["lsS"] = f(np.broadcast_to(ls[g_S][:, :, None], (2, 16, 64)).transpose(0, 2, 1).reshape(128, 16))

    def padS(arr_gph):
        o = np.zeros((2, 64, 16, 2, 16), np.float32)
        for m in range(2):
            o[m, :, :, m, :] = arr_gph[g_S[m]].transpose(1, 0, 2)
        return o.reshape(128, 16, 32)
    out["cSr"] = padS(cr.transpose(0, 2, 1)); out["cSi"] = padS(ci.transpose(0, 2, 1))
    out["bSr"] = padS(br); out["bSi"] = padS(bi)
    a_, ch_, m_ = np.arange(4), np.arange(4), np.arange(2)
    gT = 2 * (4 * ch_[None, :, None] + a_[:, None, None]) + m_[None, None, :]

    def repT(arr_gp):
        v = arr_gp[gT]
        v = np.broadcast_to(v[:, None, None], (4, 2, 16, 4, 2, 64))
        return f(v.reshape(128, 4, 128))
    out["lrT"] = repT(lr); out["liT"] = repT(li)
    out["lsT"] = repT(np.broadcast_to(ls[:, None], (32, 64)))

    def padT(arr_gph):
        o = np.zeros((4, 2, 16, 4, 2, 64), np.float32)
        for a in range(4):
            for ch in range(4):
                for m in range(2):
                    g = 2 * (4 * ch + a) + m
                    o[a, m, :, ch, m, :] = arr_gph[g].T
        return o.reshape(128, 4, 128)
    out["bTr"] = padT(br); out["bTi"] = padT(bi)
    dT = np.zeros((4, 2, 16, 4), np.float32)
    for a in range(4):
        for ch in range(4):
            for m in range(2):
                dT[a, m, :, ch] = dd[2 * (4 * ch + a) + m]
    out["dT"] = dT.reshape(128, 4)
    out["ident"] = np.eye(128, dtype=np.float32)
    out["tri"] = np.triu(np.ones((128, 128), np.float32), 1)
    pos = np.arange(1, 17, dtype=np.float32)
    invc = np.stack([1.0 / np.minimum(pos, float(w)) for w in (2, 4, 8, 16)], 0)
    out["invc"] = f(np.broadcast_to(invc[None], (128, 4, 16)))
    out["eoff1"] = f(np.broadcast_to((np.arange(32, dtype=np.float32) * cap + 1.0)[None], (128, 32)))
    return out


def core_inputs(x_c, p_c):
    ns, L = x_c.shape[0], x_c.shape[1]
    ntok = ns * L
    perm = lambda a: a.reshape(ns, L // 256, 256, a.shape[-1]).transpose(1, 0, 2, 3).reshape(ntok, a.shape[-1])
    xT = np.ascontiguousarray(perm(x_c).T).reshape(8, 128, ntok)
    pT = np.ascontiguousarray(perm(p_c).T).reshape(2, 128, ntok)
    return {"xT": xT, "pT": pT}


def core_output(oT, ns, L):
    o = np.ascontiguousarray(oT.reshape(1024, ns * L).T)
    return np.ascontiguousarray(o.reshape(L // 256, ns, 256, 1024).transpose(1, 0, 2, 3)).reshape(ns, L, 1024)


def kernel(**inputs):
    I = {k: np.asarray(v) for k, v in inputs.items()}
    x, pp = I["x"], I["p"][0]
    B, L, Dm = x.shape
    ncore = 8
    nseq = B // ncore
    shared = shared_layout(I)
    nc = build(nseq, L)
    in_maps = []
    for c in range(ncore):
        m = dict(shared)
        m.update(core_inputs(x[c * nseq:(c + 1) * nseq], pp[c * nseq:(c + 1) * nseq]))
        in_maps.append(m)
    res = run_bass_kernel_spmd(nc, in_maps, core_ids=list(range(ncore)))
    outs = []
    for c in range(ncore):
        outs.append(core_output(np.asarray(res.results[c]["outT"]), nseq, L))
    return np.concatenate(outs, axis=0).astype(np.float32)
```

```python
import types
import numpy as np
from contextlib import ExitStack
import concourse.bass as bass
import concourse.mybir as mybir
from concourse.bass_utils import run_bass_kernel_spmd

F32 = mybir.dt.float32
BF16 = mybir.dt.bfloat16
I32 = mybir.dt.int32
AF = mybir.ActivationFunctionType
ALU = mybir.AluOpType
AX = mybir.AxisListType

ENGS = ["sync", "act", "dve", "pool", "pe"]
NT = 512
ALPHA = float(2.0 ** 0.25)
TWO_PI = float(2 * np.pi)


def _snap(fn):
    if fn is None or fn.__closure__ is None:
        return fn
    cells = []
    for c in fn.__closure__:
        try:
            cells.append(types.CellType(c.cell_contents))
        except ValueError:
            cells.append(c)
    g = types.FunctionType(fn.__code__, fn.__globals__, fn.__name__, fn.__defaults__, tuple(cells))
    g.__kwdefaults__ = fn.__kwdefaults__
    return g


class Prog:
    def __init__(self, nc, es):
        self.nc = nc
        self.es = es
        self.ops = {e: [] for e in ENGS}
        self.cnt = {e: 0 for e in ENGS}
        self.esem = {}
        for e in ["act", "dve", "pool", "pe"]:
            self.esem[e] = es.enter_context(nc.semaphore("s_" + e))
        self.dsem = {}
        self.last_w = {}
        self.readers = {}
        self.seen = {e: {} for e in ENGS}

    def _need(self, eng, ev, waits):
        if ev is None:
            return
        sem, val, src = ev
        if src == "pe" and eng == "pe":
            return
        k = id(sem)
        cur = self.seen[eng].get(k, (None, 0))[1]
        if cur >= val:
            return
        self.seen[eng][k] = (sem, val)
        waits[k] = (sem, max(val, waits.get(k, (None, 0))[1]))

    def _deps(self, eng, reads, writes):
        waits = {}
        for k in reads:
            self._need(eng, self.last_w.get(k), waits)
        for k in writes:
            self._need(eng, self.last_w.get(k), waits)
            for r in self.readers.get(k, []):
                self._need(eng, r, waits)
        return list(waits.values())

    def _commit(self, ev, reads, writes):
        for k in reads:
            self.readers.setdefault(k, []).append(ev)
        for k in writes:
            self.last_w[k] = ev
            self.readers[k] = []

    def op(self, eng, fn, reads=(), writes=()):
        waits = self._deps(eng, reads, writes)
        self.cnt[eng] += 1
        ev = (self.esem[eng], self.cnt[eng], eng)
        self.ops[eng].append((waits, _snap(fn), [(self.esem[eng], 1)]))
        self._commit(ev, reads, writes)

    def dma(self, eng, fns, reads=(), writes=(), key=None):
        if key is None:
            key = writes[0]
        if key not in self.dsem:
            self.dsem[key] = [self.es.enter_context(self.nc.semaphore("d%d" % len(self.dsem))), 0]
        ent = self.dsem[key]
        waits = self._deps(eng, reads, writes)
        for i, fn in enumerate(fns):
            ent[1] += 16
            self.ops[eng].append((waits if i == 0 else [], _snap(fn), [(ent[0], 16)]))
        ev = (ent[0], ent[1], "dma")
        self._commit(ev, reads, writes)

    def seal(self, key, keys):
        ent = self.dsem[key]
        for k in keys:
            self.last_w[k] = (ent[0], ent[1], "dma")

    def barrier(self):
        evs = [(self.esem[x], self.cnt[x], x) for x in ["act", "dve", "pool", "pe"] if self.cnt[x] > 0]
        evs += [(ent[0], ent[1], "dma") for k_, ent in self.dsem.items() if ent[1] > 0 and not str(k_).startswith("prep")]
        for eng in ENGS:
            waits = {}
            for sem, val, src in evs:
                kk = id(sem)
                if self.seen[eng].get(kk, (None, 0))[1] >= val:
                    continue
                self.seen[eng][kk] = (sem, val)
                waits[kk] = (sem, val)
            if waits:
                self.ops[eng].append((list(waits.values()), None, []))

    def final_wait(self, eng, keys):
        waits = self._deps(eng, keys, [])
        self.ops[eng].append((waits, None, []))

    def emit(self, block):
        def run(e, lst):
            for waits, fn, incs in lst:
                for sem, val in waits:
                    e.wait_ge(sem, val)
                if fn is None:
                    continue
                ins = fn(e)
                for sem, v in incs:
                    ins.then_inc(sem, v)

        @block.sync
        def _(e):
            run(e, self.ops["sync"])

        @block.scalar
        def _(e):
            run(e, self.ops["act"])

        @block.vector
        def _(e):
            run(e, self.ops["dve"])

        @block.gpsimd
        def _(e):
            run(e, self.ops["pool"])

        @block.tensor
        def _(e):
            run(e, self.ops["pe"])


IN_SPECS = None


def in_specs(ntok):
    return {
        "xT": ([8, 128, ntok], F32), "pT": ([2, 128, ntok], F32),
        "w_in": ([8, 128, 3072], F32), "w_val": ([4, 128, 1024], F32), "w_gg": ([4, 128, 1024], F32),
        "w_pp": ([4, 128, 1024], F32), "w_out": ([8, 128, 1024], F32),
        "w_plg": ([8, 128, 1024], F32), "w_plp": ([2, 128, 1024], F32),
        "w_pg": ([128, 4, 128], F32), "pscale": ([128, 4], F32),
        "ln1g": ([128, 8], F32), "ln1b": ([128, 8], F32), "ln2g": ([128, 8], F32), "ln2b": ([128, 8], F32),
        "w_r": ([128, 8, 32], F32), "b_r": ([128, 32], F32),
        "w_eg": ([32, 8, 128, 1024], F32), "w_eu": ([32, 8, 128, 1024], F32), "w_ed": ([32, 8, 128, 1024], F32),
        "b_eg": ([128, 32, 8], F32), "b_eu": ([128, 32, 8], F32),
        "lrS": ([128, 16], F32), "liS": ([128, 16], F32), "lsS": ([128, 16], F32),
        "cSr": ([128, 16, 32], F32), "cSi": ([128, 16, 32], F32),
        "bSr": ([128, 16, 32], F32), "bSi": ([128, 16, 32], F32),
        "lrT": ([128, 4, 128], F32), "liT": ([128, 4, 128], F32), "lsT": ([128, 4, 128], F32),
        "bTr": ([128, 4, 128], F32), "bTi": ([128, 4, 128], F32), "dT": ([128, 4], F32),
        "ident": ([128, 128], F32), "invc": ([128, 4, 16], F32), "tri": ([128, 128], F32), "eoff1": ([128, 32], F32),
        "b_down": ([32, 1024], F32),
    }


def build(nseq, seqlen, n_experts=32, dbg=False, cap=1536):
    assert nseq == 2
    ntok = nseq * seqlen
    ntiles = ntok // NT
    HS = NT // 2
    nc = bass.Bass("TRN2", target_bir_lowering=False)
    D = {}
    for name, (shape, dt) in in_specs(ntok).items():
        D[name] = nc.dram_tensor(name, shape, dt, kind="ExternalInput").ap()
    outT = nc.dram_tensor("outT", [8, 128, ntok], F32, kind="ExternalOutput").ap()
    h1T = nc.dram_tensor("h1T", [8, 128, ntok], F32, kind="ExternalOutput" if dbg else "Internal").ap()
    NSLOT = 32 * cap
    NBLK = cap // 128
    NBLKT = ntok // 128
    h1tm16 = nc.dram_tensor("h1tm16", [ntok + 1, 1024], BF16, kind="Internal").ap()
    wtab = nc.dram_tensor("wtab", [ntok + 1, 32], F32, kind="Internal").ap()
    toklist = nc.dram_tensor("toklist", [NSLOT, 1], I32, kind="Internal").ap()
    ysd = nc.dram_tensor("ysd", [NSLOT + 1, 1024], F32, kind="Internal").ap()
    S = {}
    for name in ["w_in", "w_val", "w_gg", "w_pp", "w_out", "w_plg", "w_plp", "w_eg", "w_eu", "w_ed"]:
        S[name] = nc.dram_tensor("s_" + name, in_specs(ntok)[name][0], BF16, kind="Internal").ap()

    with ExitStack() as es:
        p = Prog(nc, es)
        T = lambda n, s, d, st=es: st.enter_context(nc.sbuf_tensor(n, s, d))
        banks = [es.enter_context(nc.psum_tensor("pb%d" % i, [128, 512], F32)) for i in range(8)]
        block = es.enter_context(nc.Block())
        bank_i = [0]

        def nb(nrot=6):
            i = bank_i[0] % nrot
            bank_i[0] += 1
            return banks[i], "pb%d" % i

        dbg_outs = {}
        bc_regs = {}

        def bcr(e, value):
            if value not in bc_regs:
                r = e.alloc_register("bc%d" % len(bc_regs))
                e.reg_mov(r, int(value))
                bc_regs[value] = r
            return bc_regs[value]

        def dump(name, tile, key, eng="sync"):
            if not dbg or name in dbg_outs:
                return
            shape = list(tile.shape)
            d = nc.dram_tensor("dbg_" + name, shape, tile.dtype, kind="ExternalOutput").ap()
            dbg_outs[name] = d
            p.dma(eng, [lambda e: e.dma_start(out=d, in_=tile[:])], reads=[key], writes=["dbg_" + name], key="dbg")

        fns = []
        for name in ["w_in", "w_val", "w_gg", "w_pp", "w_out", "w_plg", "w_plp"]:
            for kc in range(D[name].shape[0]):
                fns.append(lambda e, name=name, kc=kc: e.dma_start(out=S[name][kc], in_=D[name][kc]))
        p.dma("pool", fns, writes=["prepA"], key="prepA")
        def prep_expert(ex):
            fns = []
            for name in ["w_eg", "w_eu", "w_ed"]:
                for kc in range(8):
                    fns.append(lambda e, name=name, kc=kc, ex=ex: e.dma_start(out=S[name][ex, kc], in_=D[name][ex, kc]))
            p.dma("pool", fns, writes=["prepE%d" % ex], key="prepE%d" % ex)
        prep_per_tile = -(-n_experts // ntiles)
        prep_next = [0]

        cst = {}
        fns = []
        for name in ["pscale", "ln1g", "ln1b", "ln2g", "ln2b", "w_r", "b_r",
                     "dT", "ident", "invc", "eoff1"]:
            shape, dt = in_specs(ntok)[name]
            cst[name] = T("c_" + name, shape, dt)
            fns.append(lambda e, name=name: e.dma_start(out=cst[name][:], in_=D[name]))
        p.dma("sync", fns, writes=["cst"], key="cst")
        p.seal("cst", ["cst"])
        DEST = T("DEST", [128, NBLKT, 4], I32)
        CNT = T("CNT", [128, 32], F32)
        TOKID = T("TOKID", [128, NBLKT], I32)
        TRI = T("TRI", [128, 128], BF16)
        p.op("pool", lambda e: e.iota(TOKID[:], pattern=[[128, NBLKT]], base=0, channel_multiplier=1), writes=["TOKID"])
        p.op("dve", lambda e: e.memset(CNT[:], 0.0), writes=["CNT"])
        with ExitStack() as stw:
            tri32 = T("tri32", [128, 128], F32, stw)
            p.dma("sync", [lambda e: e.dma_start(out=tri32[:], in_=D["tri"])], writes=["tri32"])
            p.op("dve", lambda e: e.tensor_copy(out=TRI[:], in_=tri32[:]), reads=["tri32"], writes=["TRI"])
            p.barrier()
        with ExitStack() as st0:
            z32 = T("z32", [1, 1024], F32, st0); z16 = T("z16", [1, 1024], BF16, st0); tli = T("tli", [128, NSLOT // 128], I32, st0)
            p.op("dve", lambda e: e.memset(z32[:], 0.0), writes=["z32"])
            p.op("dve", lambda e: e.memset(z16[:], 0.0), writes=["z16"])
            p.op("dve", lambda e: e.memset(tli[:], ntok), writes=["tli"])
            p.dma("sync", [lambda e: e.dma_start(out=h1tm16[ntok:ntok + 1, :], in_=z16[:]),
                           lambda e: e.dma_start(out=wtab[ntok:ntok + 1, :], in_=z32[:, 0:32]),
                           lambda e: e.dma_start(out=ysd[NSLOT:NSLOT + 1, :], in_=z32[:]),
                           lambda e: e.dma_start(out=toklist.rearrange("(p a) o -> p (a o)", p=128), in_=tli[:])],
                  reads=["z32", "z16", "tli"], writes=["init"], key="init")
            p.seal("init", ["init"])
            p.barrier()
        identb = T("identb", [128, 128], BF16)
        onesb = T("onesb", [128, 128], BF16)
        wpgb = T("wpgb", [128, 4, 128], BF16)
        p.op("dve", lambda e: e.tensor_copy(out=identb[:], in_=cst["ident"][:]), reads=["cst"], writes=["identb"])
        p.op("dve", lambda e: e.memset(onesb[:], 1.0), writes=["onesb"])
        with ExitStack() as stw:
            wpg32 = T("wpg32", [128, 4, 128], F32, stw)
            p.dma("sync", [lambda e: e.dma_start(out=wpg32[:], in_=D["w_pg"])], writes=["wpg32"])
            p.op("dve", lambda e: e.tensor_copy(out=wpgb[:], in_=wpg32[:]), reads=["wpg32"], writes=["wpgb"])
            p.barrier()

        with ExitStack() as esA:
            TA = lambda n, s, d: T(n, s, d, esA)
            WT = TA("WT", [128, 4, 16, 2, 128], BF16)
            GS = TA("GS", [128, 16, 16, 2, 32], BF16)
            KL = TA("KL", [128, 4, 16, 128], BF16)
            DG = TA("DG", [128, 4, 128], BF16)
            AR = TA("AR", [128, 16], F32)
            AI = TA("AI", [128, 16], F32)

            def powers(pre, tl, lr, li, ls, F, K, st, kpre=None):
                TS = lambda n, s, d: T(pre + n, s, d, st)
                s_ = TS("s", [128, F], F32); a_ = TS("a", [128, F], F32); th = TS("th", [128, F], F32)
                MAG = TS("MAG", [128, K + 1, F], F32); TT = TS("TT", [128, K + 1, F], F32)
                W1 = TS("W1", [128, K + 1, F], F32); I1 = TS("I1", [128, K + 1, F], I32)
                W2 = TS("W2", [128, K + 1, F], F32)
                PWR = TS("PWR", [128, K + 1, F], F32); PWI = TS("PWI", [128, K + 1, F], F32)
                k = lambda n: (kpre or pre) + n
                p.op("act", lambda e: e.activation(out=s_[:], in_=ls, func=AF.Exp), reads=[tl], writes=[k("s")])
                p.op("dve", lambda e: e.tensor_tensor(out=a_[:], in0=lr, in1=s_[:], op=ALU.mult), reads=[tl, k("s")], writes=[k("a")])
                p.op("dve", lambda e: e.tensor_tensor(out=th[:], in0=li, in1=s_[:], op=ALU.mult), reads=[tl, k("s")], writes=[k("th")])
                p.op("dve", lambda e: e.tensor_scalar(out=th[:], in0=th[:], scalar1=float(1.0 / TWO_PI), scalar2=None, op0=ALU.mult), reads=[k("th")], writes=[k("th")])
                for kk in range(K + 1):
                    p.op("act", lambda e, kk=kk: e.activation(out=MAG[:, kk, :], in_=a_[:], func=AF.Exp, scale=float(kk)), reads=[k("a")], writes=[k("MAG")])
                    p.op("dve", lambda e, kk=kk: e.tensor_scalar(out=TT[:, kk, :], in0=th[:], scalar1=float(kk), scalar2=None, op0=ALU.mult), reads=[k("th")], writes=[k("TT")])
                for off, OUT, on in [(0.25, PWR, "PWR"), (0.0, PWI, "PWI")]:
                    p.op("dve", lambda e, off=off: e.tensor_scalar(out=W1[:], in0=TT[:], scalar1=float(off), scalar2=None, op0=ALU.add), reads=[k("TT")], writes=[k("W1")])
                    p.op("dve", lambda e: e.tensor_copy(out=I1[:], in_=W1[:]), reads=[k("W1")], writes=[k("I1")])
                    p.op("dve", lambda e: e.tensor_copy(out=W2[:], in_=I1[:]), reads=[k("I1")], writes=[k("W2")])
                    p.op("dve", lambda e: e.tensor_sub(out=W1[:], in0=W1[:], in1=W2[:]), reads=[k("W1"), k("W2")], writes=[k("W1")])
                    p.op("dve", lambda e: e.tensor_single_scalar(out=W2[:], in_=W1[:], scalar=0.5, op=ALU.is_gt), reads=[k("W1")], writes=[k("W2")])
                    p.op("dve", lambda e: e.tensor_sub(out=W1[:], in0=W1[:], in1=W2[:]), reads=[k("W1"), k("W2")], writes=[k("W1")])
                    p.op("dve", lambda e: e.tensor_single_scalar(out=W2[:], in_=W1[:], scalar=-0.5, op=ALU.is_lt), reads=[k("W1")], writes=[k("W2")])
                    p.op("dve", lambda e: e.tensor_add(out=W1[:], in0=W1[:], in1=W2[:]), reads=[k("W1"), k("W2")], writes=[k("W1")])
                    p.op("act", lambda e, OUT=OUT: e.activation(out=OUT[:], in_=W1[:], func=AF.Sin, scale=TWO_PI), reads=[k("W1")], writes=[k(on)])
                    p.op("dve", lambda e, OUT=OUT: e.tensor_mul(out=OUT[:], in0=OUT[:], in1=MAG[:]), reads=[k(on), k("MAG")], writes=[k(on)])
                n1 = TS("n1", [128, F], F32); n2 = TS("n2", [128, F], F32); n3 = TS("n3", [128, F], F32)
                cr = TS("cr", [128, F], F32); ci = TS("ci", [128, F], F32)
                p.op("dve", lambda e: e.tensor_scalar(out=n1[:], in0=PWR[:, 1, :], scalar1=-1.0, scalar2=None, op0=ALU.add), reads=[k("PWR")], writes=[k("n1")])
                p.op("dve", lambda e: e.tensor_mul(out=n2[:], in0=n1[:], in1=lr), reads=[k("n1"), tl], writes=[k("n2")])
                p.op("dve", lambda e: e.tensor_mul(out=n3[:], in0=PWI[:, 1, :], in1=li), reads=[k("PWI"), tl], writes=[k("n3")])
                p.op("dve", lambda e: e.tensor_add(out=cr[:], in0=n2[:], in1=n3[:]), reads=[k("n2"), k("n3")], writes=[k("cr")])
                p.op("dve", lambda e: e.tensor_mul(out=n2[:], in0=PWI[:, 1, :], in1=lr), reads=[k("PWI"), tl, k("cr")], writes=[k("n2")])
                p.op("dve", lambda e: e.tensor_mul(out=n3[:], in0=n1[:], in1=li), reads=[k("n1"), tl, k("cr")], writes=[k("n3")])
                p.op("dve", lambda e: e.tensor_sub(out=ci[:], in0=n2[:], in1=n3[:]), reads=[k("n2"), k("n3")], writes=[k("ci")])
                p.op("dve", lambda e: e.tensor_mul(out=n2[:], in0=lr, in1=lr), reads=[tl, k("ci")], writes=[k("n2")])
                p.op("dve", lambda e: e.tensor_mul(out=n3[:], in0=li, in1=li), reads=[tl, k("ci")], writes=[k("n3")])
                p.op("dve", lambda e: e.tensor_add(out=n2[:], in0=n2[:], in1=n3[:]), reads=[k("n2"), k("n3")], writes=[k("n2")])
                p.op("dve", lambda e: e.reciprocal(out=n2[:], in_=n2[:]), reads=[k("n2")], writes=[k("n2")])
                p.op("dve", lambda e: e.tensor_mul(out=cr[:], in0=cr[:], in1=n2[:]), reads=[k("cr"), k("n2")], writes=[k("cr")])
                p.op("dve", lambda e: e.tensor_mul(out=ci[:], in0=ci[:], in1=n2[:]), reads=[k("ci"), k("n2")], writes=[k("ci")])
                return PWR, PWI, cr, ci

            for ch in range(4):
                with ExitStack() as st:
                    pre = "T%d_" % ch
                    TS = lambda n, s, d: T(pre + n, s, d, st)
                    inT = TS("in", [128, 5, 128], F32)
                    p.dma("sync", [lambda e, i=i, nm=nm, ch=ch: e.dma_start(out=inT[:, i, :], in_=D[nm][:, ch, :])
                                   for i, nm in enumerate(["lrT", "liT", "lsT", "bTr", "bTi"])], writes=["T_in"], key="ldT")
                    PWR, PWI, cr, ci = powers(pre, "T_in", inT[:, 0, :], inT[:, 1, :], inT[:, 2, :], 128, 15, st, "T_")
                    bbr = TS("bbr", [128, 128], F32); bbi = TS("bbi", [128, 128], F32)
                    t1 = TS("t1", [128, 16, 128], F32); t2 = TS("t2", [128, 16, 128], F32)
                    k = lambda n: "T_" + n
                    p.op("dve", lambda e: e.tensor_mul(out=t1[:, 0, :], in0=cr[:], in1=inT[:, 3, :]), reads=[k("cr"), k("in")], writes=[k("t1")])
                    p.op("dve", lambda e: e.tensor_mul(out=t2[:, 0, :], in0=ci[:], in1=inT[:, 4, :]), reads=[k("ci"), k("in")], writes=[k("t2")])
                    p.op("dve", lambda e: e.tensor_sub(out=bbr[:], in0=t1[:, 0, :], in1=t2[:, 0, :]), reads=[k("t1"), k("t2")], writes=[k("bbr")])
                    p.op("dve", lambda e: e.tensor_mul(out=t1[:, 0, :], in0=cr[:], in1=inT[:, 4, :]), reads=[k("cr"), k("in"), k("bbr")], writes=[k("t1")])
                    p.op("dve", lambda e: e.tensor_mul(out=t2[:, 0, :], in0=ci[:], in1=inT[:, 3, :]), reads=[k("ci"), k("in"), k("bbr")], writes=[k("t2")])
                    p.op("dve", lambda e: e.tensor_add(out=bbi[:], in0=t1[:, 0, :], in1=t2[:, 0, :]), reads=[k("t1"), k("t2")], writes=[k("bbi")])
                    bc = lambda t: t[:].unsqueeze(1).to_broadcast([128, 16, 128])
                    p.op("dve", lambda e: e.tensor_tensor(out=t1[:], in0=PWR[:], in1=bc(bbr), op=ALU.mult), reads=[k("PWR"), k("bbr"), k("bbi")], writes=[k("t1")])
                    p.op("dve", lambda e: e.tensor_tensor(out=t2[:], in0=PWI[:], in1=bc(bbi), op=ALU.mult), reads=[k("PWI"), k("bbi")], writes=[k("t2")])
                    p.op("dve", lambda e, ch=ch: e.tensor_sub(out=WT[:, ch, :, 0, :], in0=t1[:], in1=t2[:]), reads=[k("t1"), k("t2")], writes=["WT"])
                    p.op("dve", lambda e: e.tensor_tensor(out=t1[:], in0=PWR[:], in1=bc(bbi), op=ALU.mult), reads=[k("PWR"), k("bbi"), "WT"], writes=[k("t1")])
                    p.op("dve", lambda e: e.tensor_tensor(out=t2[:], in0=PWI[:], in1=bc(bbr), op=ALU.mult), reads=[k("PWI"), k("bbr"), "WT"], writes=[k("t2")])
                    p.op("dve", lambda e, ch=ch: e.tensor_add(out=WT[:, ch, :, 1, :], in0=t1[:], in1=t2[:]), reads=[k("t1"), k("t2")], writes=["WT"])
                    p.op("dve", lambda e, ch=ch: e.tensor_scalar(out=DG[:, ch, :], in0=cst["ident"][:], scalar1=cst["dT"][:, ch:ch + 1], scalar2=None, op0=ALU.mult), reads=["cst"], writes=["DG"])

            p.barrier()
            with ExitStack() as st:
                pre = "S_"
                TS = lambda n, s, d: T(pre + n, s, d, st)
                k = lambda n: pre + n
                inS = TS("in", [128, 3, 16], F32)
                cS = TS("c", [128, 2, 16, 32], F32)
                bS = TS("b", [128, 2, 16, 32], F32)
                p.dma("sync", [lambda e, i=i, nm=nm: e.dma_start(out=inS[:, i, :], in_=D[nm]) for i, nm in enumerate(["lrS", "liS", "lsS"])]
                      + [lambda e, i=i, nm=nm: e.dma_start(out=cS[:, i], in_=D[nm]) for i, nm in enumerate(["cSr", "cSi"])]
                      + [lambda e, i=i, nm=nm: e.dma_start(out=bS[:, i], in_=D[nm]) for i, nm in enumerate(["bSr", "bSi"])],
                      writes=[k("in")], key="ldS")
                p.seal("ldS", [k("in")])
                PWR, PWI, cr, ci = powers(pre, k("in"), inS[:, 0, :], inS[:, 1, :], inS[:, 2, :], 16, 16, st)
                p.op("dve", lambda e: e.tensor_copy(out=AR[:], in_=PWR[:, 16, :]), reads=[k("PWR")], writes=["AR"])
                p.op("dve", lambda e: e.tensor_copy(out=AI[:], in_=PWI[:, 16, :]), reads=[k("PWI")], writes=["AI"])
                bb = TS("bb", [128, 2, 16, 32], F32)
                ncSi = TS("ncSi", [128, 16, 32], F32)
                u1 = TS("u1", [128, 16, 32], F32); u2 = TS("u2", [128, 16, 32], F32)
                bq = lambda t: t.unsqueeze(2).to_broadcast([128, 16, 32])
                p.op("dve", lambda e: e.tensor_tensor(out=u1[:], in0=bS[:, 0], in1=bq(cr[:]), op=ALU.mult), reads=[k("in"), k("cr")], writes=[k("u1")])
                p.op("dve", lambda e: e.tensor_tensor(out=u2[:], in0=bS[:, 1], in1=bq(ci[:]), op=ALU.mult), reads=[k("in"), k("ci")], writes=[k("u2")])
                p.op("dve", lambda e: e.tensor_sub(out=bb[:, 0], in0=u1[:], in1=u2[:]), reads=[k("u1"), k("u2")], writes=[k("bb")])
                p.op("dve", lambda e: e.tensor_tensor(out=u1[:], in0=bS[:, 1], in1=bq(cr[:]), op=ALU.mult), reads=[k("in"), k("cr"), k("bb")], writes=[k("u1")])
                p.op("dve", lambda e: e.tensor_tensor(out=u2[:], in0=bS[:, 0], in1=bq(ci[:]), op=ALU.mult), reads=[k("in"), k("ci"), k("bb")], writes=[k("u2")])
                p.op("dve", lambda e: e.tensor_add(out=bb[:, 1], in0=u1[:], in1=u2[:]), reads=[k("u1"), k("u2")], writes=[k("bb")])
                p.op("dve", lambda e: e.tensor_scalar(out=ncSi[:], in0=cS[:, 1], scalar1=-1.0, scalar2=None, op0=ALU.mult), reads=[k("in")], writes=[k("ncSi")])
                for j in range(16):
                    pr = lambda j=j: bq(PWR[:, j + 1, :]); pi = lambda j=j: bq(PWI[:, j + 1, :])
                    p.op("dve", lambda e, pr=pr: e.tensor_tensor(out=u1[:], in0=cS[:, 0], in1=pr(), op=ALU.mult), reads=[k("in"), k("PWR"), k("bb"), "GS"], writes=[k("u1")])
                    p.op("dve", lambda e, pi=pi: e.tensor_tensor(out=u2[:], in0=cS[:, 1], in1=pi(), op=ALU.mult), reads=[k("in"), k("PWI"), k("bb"), "GS"], writes=[k("u2")])
                    p.op("dve", lambda e, j=j: e.tensor_sub(out=GS[:, :, j, 0, :], in0=u1[:], in1=u2[:]), reads=[k("u1"), k("u2")], writes=["GS"])
                    p.op("dve", lambda e, pi=pi: e.tensor_tensor(out=u1[:], in0=cS[:, 0], in1=pi(), op=ALU.mult), reads=[k("in"), k("PWI"), "GS"], writes=[k("u1")])
                    p.op("dve", lambda e, pr=pr: e.tensor_tensor(out=u2[:], in0=cS[:, 1], in1=pr(), op=ALU.mult), reads=[k("in"), k("PWR"), "GS"], writes=[k("u2")])
                    p.op("dve", lambda e, j=j: e.scalar_tensor_tensor(out=GS[:, :, j, 1, :], in0=u1[:], scalar=-1.0, in1=u2[:], op0=ALU.mult, op1=ALU.subtract), reads=[k("u1"), k("u2")], writes=["GS"])
                ES = TS("ES", [128, 2, 2, 16, 32], F32)
                p.op("dve", lambda e: e.memset(KL[:], 0.0), writes=["KL"])
                for kk in range(16):
                    sl = kk % 2
                    ek = k("ES%d" % sl)
                    pr = lambda kk=kk: bq(PWR[:, kk, :]); pi = lambda kk=kk: bq(PWI[:, kk, :])
                    p.op("dve", lambda e, pr=pr: e.tensor_tensor(out=u1[:], in0=bb[:, 0], in1=pr(), op=ALU.mult), reads=[k("bb"), k("PWR"), "GS", ek], writes=[k("u1")])
                    p.op("dve", lambda e, pi=pi: e.tensor_tensor(out=u2[:], in0=bb[:, 1], in1=pi(), op=ALU.mult), reads=[k("bb"), k("PWI"), "GS", ek], writes=[k("u2")])
                    p.op("dve", lambda e, sl=sl: e.tensor_sub(out=ES[:, sl, 0], in0=u1[:], in1=u2[:]), reads=[k("u1"), k("u2")], writes=[ek])
                    p.op("dve", lambda e, pi=pi: e.tensor_tensor(out=u1[:], in0=bb[:, 0], in1=pi(), op=ALU.mult), reads=[k("bb"), k("PWI"), ek], writes=[k("u1")])
                    p.op("dve", lambda e, pr=pr: e.tensor_tensor(out=u2[:], in0=bb[:, 1], in1=pr(), op=ALU.mult), reads=[k("bb"), k("PWR"), ek], writes=[k("u2")])
                    p.op("dve", lambda e, sl=sl: e.tensor_add(out=ES[:, sl, 1], in0=u1[:], in1=u2[:]), reads=[k("u1"), k("u2")], writes=[ek])

                    def mmk(e, kk=kk, sl=sl):
                        ins = None
                        for q in range(16):
                            ch, a = q // 4, q % 4
                            o = banks[ch][32 * a:32 * a + 32, kk * 32:(kk + 1) * 32]
                            e.matmul(o, lhsT=ES[:, sl, 0, q, :], rhs=cS[:, 0, q, :], start=True, stop=False,
                                     skip_group_check=True, tile_position=(0, 32 * a))
                            ins = e.matmul(o, lhsT=ES[:, sl, 1, q, :], rhs=ncSi[:, q, :], start=False, stop=True,
                                           skip_group_check=True, tile_position=(0, 32 * a))
                        return ins
                    p.op("pe", mmk, reads=[ek, k("in"), k("ncSi")], writes=["pb0", "pb1", "pb2", "pb3"])
                for ch in range(4):
                    for a in range(4):
                        p.op("dve", lambda e, ch=ch, a=a: e.tensor_copy(
                            out=KL[32 * a:32 * a + 32, ch, :, 32 * a:32 * a + 32],
                            in_=banks[ch][32 * a:32 * a + 32, :].rearrange("p (k c) -> p k c", c=32)),
                            reads=["pb%d" % ch], writes=["KL"])

            p.barrier()
            dump("WT", WT, "WT"); dump("GS", GS, "GS"); dump("KL", KL, "KL"); dump("AR", AR, "AR"); dump("AI", AI, "AI"); dump("DG", DG, "DG")
            NB = 4
            wbuf = [TA("wbuf%d" % i, [128, 8, 512], BF16) for i in range(NB)]
            wb_i = [0]

            def load_block(name, kcs, c0, ncol):
                i = wb_i[0] % NB
                wb_i[0] += 1
                buf = wbuf[i]
                view = buf[:].rearrange("p a b -> p (a b)")[:, 0:kcs * ncol].rearrange("p (a b) -> p a b", b=ncol)
                p.dma("sync", [lambda e: e.dma_start(out=view, in_=S[name][:, :, c0:c0 + ncol].rearrange("k p n -> p k n"))],
                      reads=["prepA"], writes=["wbuf%d" % i])
                return view, "wbuf%d" % i

            x32_ = [TA("x32_%d" % i, [128, 8, NT], F32) for i in range(2)]
            xb = TA("xb", [128, 8, NT], BF16)
            us = TA("us", [128, 4, 16, 32], BF16)
            up = TA("up", [128, 4, 2, HS + 16], F32)
            XLs = TA("XLs", [128, 2, 16, 32], F32)
            XP = TA("XP", [128, 2, 16, 32], BF16)
            Z4 = TA("Z4", [128, 4, 32], F32)
            C1 = TA("C1", [128, 2, 16, 2], F32); C2 = TA("C2", [128, 2, 16, 2], F32)
            rt = [TA("rt%d" % i, [128, 2, 32], F32) for i in range(2)]
            bqs = lambda t_: t_[:].unsqueeze(2).to_broadcast([128, 16, 2])
            p.op("dve", lambda e: e.tensor_copy(out=C1[:, 0], in_=bqs(AR)), reads=["AR"], writes=["C12"])
            p.op("dve", lambda e: e.tensor_copy(out=C1[:, 1], in_=bqs(AR)), reads=["AR", "C12"], writes=["C12"])
            p.op("dve", lambda e: e.tensor_copy(out=C2[:, 1], in_=bqs(AI)), reads=["AI", "C12"], writes=["C12"])
            p.op("dve", lambda e: e.tensor_scalar(out=C2[:, 0], in0=C2[:, 1], scalar1=-1.0, scalar2=None, op0=ALU.mult), reads=["C12"], writes=["C12"])
            zb = TA("zb", [128, 4, NT], BF16)
            plb = zb
            mixb = TA("mixb", [128, 4, NT], BF16)
            merged = TA("merged", [128, 8, NT], BF16)
            mb = merged
            sA = TA("sA", [128, 2, HS + 16], F32); sB = TA("sB", [128, 2, HS + 16], F32)
            tmp = [TA("tmp%d" % i, [128, NT], F32) for i in range(3)]
            tmp_i = [0]

            def nt_():
                i = tmp_i[0] % 3
                tmp_i[0] += 1
                return tmp[i], "tmp%d" % i
            lnm, lnr = tmp[0], tmp[1]
            sqb = merged
            lg = TA("lg", [128, 4, 32], F32); mx8 = TA("mx8", [128, 4, 8], F32); nmx = TA("nmx", [128, 4], F32)
            msk = TA("msk", [128, 4, 32], F32); ssum = TA("ssum", [128, 4], F32); msk16 = TA("msk16", [128, 4, 32], BF16)
            pos = TA("pos", [128, 4, 32], F32); gq = TA("gq", [128, 4, 32], F32); lgw = TA("lgw", [128, 4, 32], F32)
            d8 = TA("d8", [128, 4, 8], F32); d4 = TA("d4", [128, 4, 4], F32)
            hT16 = [tmp[1], tmp[2]]

            ln_state = {}

            def layer_norm(src, skey, gname, bname, xbf, xbkey, part):
                if part == "b":
                    b1, k1, b2, k2 = ln_state["banks"]
                if part == "a":
                    p.op("dve", lambda e: e.tensor_copy(out=xbf[:], in_=src[:]), reads=[skey], writes=[xbkey])
                    b1, k1 = nb()

                    def mm1(e):
                        for kc in range(8):
                            ins = e.matmul(b1[:], lhsT=onesb[:], rhs=xbf[:, kc, :], start=(kc == 0), stop=(kc == 7))
                        return ins
                    p.op("pe", mm1, reads=[xbkey, "onesb"], writes=[k1])
                    p.op("act", lambda e: e.activation(out=sqb[:], in_=src[:], func=AF.Square), reads=[skey], writes=["merged"])
                    b2, k2 = nb()

                    def mm2(e):
                        for kc in range(8):
                            ins = e.matmul(b2[:], lhsT=onesb[:], rhs=sqb[:, kc, :], start=(kc == 0), stop=(kc == 7))
                        return ins
                    p.op("pe", mm2, reads=["merged", "onesb"], writes=[k2])
                    ln_state["banks"] = (b1, k1, b2, k2)
                    return
                p.op("act", lambda e: e.activation(out=lnm[:], in_=b1[:], func=AF.Copy, scale=1.0 / 1024), reads=[k1], writes=["tmp0"])
                p.op("dve", lambda e: e.tensor_mul(out=lnr[:], in0=lnm[:], in1=lnm[:]), reads=["tmp0"], writes=["tmp1"])
                p.op("dve", lambda e: e.scalar_tensor_tensor(out=lnr[:], in0=b2[:], scalar=1.0 / 1024, in1=lnr[:], op0=ALU.mult, op1=ALU.subtract), reads=[k2, "tmp1"], writes=["tmp1"])
                p.op("act", lambda e: e.activation(out=lnr[:], in_=lnr[:], func=AF.Sqrt, bias=1e-5, scale=1.0), reads=["tmp1"], writes=["tmp1"])
                p.op("dve", lambda e: e.reciprocal(out=lnr[:], in_=lnr[:]), reads=["tmp1"], writes=["tmp1"])
                bc8 = lambda t: t[:].unsqueeze(1).to_broadcast([128, 8, NT])
                p.op("dve", lambda e: e.tensor_tensor(out=src[:], in0=src[:], in1=bc8(lnm), op=ALU.subtract), reads=[skey, "tmp0"], writes=[skey])
                p.op("dve", lambda e: e.tensor_tensor(out=src[:], in0=src[:], in1=bc8(lnr), op=ALU.mult), reads=[skey, "tmp1"], writes=[skey])
                for kc in range(8):
                    p.op("act", lambda e, kc=kc: e.activation(out=src[:, kc, :], in_=src[:, kc, :], func=AF.Identity,
                                                              scale=cst[gname][:, kc:kc + 1], bias=cst[bname][:, kc:kc + 1]),
                         reads=[skey, "cst"], writes=[skey])
            print("phase A sbuf remaining", nc.sbuf_bytes_remaining)

            def front(t, part):
                tok0 = t * NT
                first = (t == 0)
                x32 = x32_[t % 2]
                kx = "x32_%d" % (t % 2)
                if part == "a":
                    p.dma("sync", [lambda e, h=h, tok0=tok0: e.dma_start(out=x32[:, 4 * h:4 * h + 4, :],
                                                                         in_=D["xT"][4 * h:4 * h + 4, :, tok0:tok0 + NT].rearrange("k p n -> p k n"))
                                   for h in range(2)], writes=[kx])
                    p.op("dve", lambda e: e.tensor_copy(out=xb[:, 0:4, :], in_=x32[:, 0:4, :]), reads=[kx], writes=["xb"])
                    p.op("act", lambda e: e.activation(out=xb[:, 4:8, :], in_=x32[:, 4:8, :], func=AF.Copy), reads=[kx, "xb"], writes=["xb"])
                    if first:
                        p.op("pool", lambda e: e.memset(up[:, :, :, 0:16], 0.0), writes=["up"])
                        p.op("pool", lambda e: e.memset(Z4[:], 0.0), writes=["Z4"])
                    return
                for cb in range(2):
                    wv, wk = load_block("w_in", 8, cb * 512, 512)
                    for mi in range(4):
                        mo = cb * 4 + mi
                        bk, bkey = nb()

                        def mm(e, wv=wv, mi=mi, bk=bk):
                            for kc in range(8):
                                ins = e.matmul(bk[:], lhsT=wv[:, kc, mi * 128:(mi + 1) * 128], rhs=xb[:, kc, :], start=(kc == 0), stop=(kc == 7))
                            return ins
                        p.op("pe", mm, reads=[wk, "xb"], writes=[bkey])
                        if mo < 4:
                            p.op("dve", lambda e, mo=mo, bk=bk: e.tensor_copy(out=us[:, mo].rearrange("p j c -> p c j"), in_=bk[:].rearrange("p (c j) -> p c j", j=16)), reads=[bkey], writes=["us"])
                        else:
                            p.op("act", lambda e, mo=mo, bk=bk: e.activation(out=up[:, mo - 4, :, 16:16 + HS], in_=bk[:].rearrange("p (s j) -> p s j", s=2), func=AF.Copy), reads=[bkey], writes=["up"])
                def mmx(e):
                    for q in range(16):
                        ch, a = q // 4, q % 4
                        for ri in range(2):
                            o = banks[6 + ri][:, q * 32:(q + 1) * 32]
                            for i in range(16):
                                ins = e.matmul(o, lhsT=WT[32 * a:32 * a + 32, ch, 15 - i, ri, :], rhs=us[32 * a:32 * a + 32, ch, i, :],
                                               start=(i == 0), stop=(i == 15), skip_group_check=True, tile_position=(32 * a, 0))
                    return ins
                p.op("pe", mmx, reads=["WT", "us"], writes=["pb6", "pb7"])
                p.op("dve", lambda e: e.tensor_copy(out=XLs[:, 0].rearrange("p cc x -> p x cc"), in_=banks[6][:].rearrange("p (x cc) -> p x cc", cc=16)), reads=["pb6"], writes=["XLs"])
                p.op("act", lambda e: e.activation(out=XLs[:, 1].rearrange("p cc x -> p x cc"), in_=banks[7][:].rearrange("p (x cc) -> p x cc", cc=16), func=AF.Copy), reads=["pb7", "XLs"], writes=["XLs"])
                dump("us", us, "us"); dump("up", up, "up"); dump("XLs", XLs, "XLs")

                W_ = HS + 16
                for g, w in enumerate((2, 4, 8, 16)):
                    p.op("pool", lambda e, g=g: e.tensor_tensor(out=sA[:, :, 1:W_], in0=up[:, g, :, 1:W_], in1=up[:, g, :, 0:W_ - 1], op=ALU.add), reads=["up", "sB"], writes=["sA"])
                    cur, curk, oth, othk = sA, "sA", sB, "sB"
                    sh = 1
                    while sh * 2 < w:
                        sh2 = sh * 2
                        lo = 2 * sh2 - 1
                        p.op("pool", lambda e, cur=cur, oth=oth, lo=lo, sh2=sh2: e.tensor_tensor(out=oth[:, :, lo:W_], in0=cur[:, :, lo:W_], in1=cur[:, :, lo - sh2:W_ - sh2], op=ALU.add),
                             reads=[curk], writes=[othk])
                        cur, curk, oth, othk = oth, othk, cur, curk
                        sh = sh2
                    plv = plb[:, g, :].rearrange("p (s j) -> p s j", s=2)
                    p.op("dve", lambda e, cur=cur, g=g, w=w, plv=plv: e.scalar_tensor_tensor(out=plv, in0=cur[:, :, 16:W_], scalar=1.0 / w, in1=up[:, g, :, 16:W_], op0=ALU.mult, op1=ALU.subtract),
                         reads=[curk, "up"], writes=["zb"])
                    if first:
                        p.op("dve", lambda e, cur=cur, g=g: e.tensor_tensor(out=cur[:, :, 16:32], in0=cur[:, :, 16:32],
                                                                             in1=cst["invc"][:, g, :].unsqueeze(1).to_broadcast([128, 2, 16]), op=ALU.mult), reads=[curk, "cst", "zb"], writes=[curk])
                        p.op("dve", lambda e, cur=cur, g=g, plv=plv: e.tensor_tensor(out=plv[:, :, 0:16], in0=cur[:, :, 16:32], in1=up[:, g, :, 16:32], op=ALU.subtract), reads=[curk, "up"], writes=["zb"])
                p.op("pool", lambda e: e.tensor_copy(out=up[:, :, :, 0:16], in_=up[:, :, :, HS:HS + 16]), reads=["up"], writes=["up"])

            def mid(t):
                tok0 = t * NT
                first = (t == 0)
                x32 = x32_[t % 2]
                kx = "x32_%d" % (t % 2)
                for g in range(4):
                    bk, bkey = nb()
                    p.op("pe", lambda e, g=g, bk=bk: e.matmul(bk[:], lhsT=wpgb[:, g, :], rhs=plb[:, g, :], start=True, stop=True), reads=["wpgb", "zb"], writes=[bkey])
                    p.op("act", lambda e, g=g, bk=bk: e.activation(out=mixb[:, g, :], in_=bk[:], func=AF.Copy, scale=cst["pscale"][:, g:g + 1]), reads=[bkey, "cst"], writes=["mixb"])
                wpv, wpk = load_block("w_pp", 4, 0, 1024)
                for mo in range(8):
                    bk, bkey = nb()

                    def mmp(e, mo=mo, bk=bk):
                        for kc in range(4):
                            ins = e.matmul(bk[:], lhsT=wpv[:, kc, mo * 128:(mo + 1) * 128], rhs=mixb[:, kc, :], start=(kc == 0), stop=(kc == 3))
                        return ins
                    if mo % 4 == 0:
                        wbv, wbk = load_block("w_in", 8, 2048 + (mo // 4) * 512, 512)
                    bb_, bbk = nb()

                    def mmb(e, mo=mo, bb_=bb_, wbv=wbv):
                        for kc in range(8):
                            ins = e.matmul(bb_[:], lhsT=wbv[:, kc, (mo % 4) * 128:(mo % 4 + 1) * 128], rhs=xb[:, kc, :], start=(kc == 0), stop=(kc == 7))
                        return ins
                    p.op("pe", mmp, reads=[wpk, "mixb"], writes=[bkey])
                    p.op("pe", mmb, reads=[wbk, "xb"], writes=[bbk])
                    t3, t3k = nt_()
                    p.op("act", lambda e, bb_=bb_, t3=t3: e.activation(out=t3[:], in_=bb_[:], func=AF.Sigmoid), reads=[bbk], writes=[t3k])
                    p.op("dve", lambda e, mo=mo, bk=bk, t3=t3: e.tensor_tensor(out=merged[:, mo, :], in0=bk[:], in1=t3[:], op=ALU.mult), reads=[bkey, t3k], writes=["merged"])
                c1v = C1[:].rearrange("p r q s -> p r (q s)"); c2v = C2[:].rearrange("p r q s -> p r (q s)")
                for cc in range(16):
                    p.op("pool", lambda e, cc=cc: e.tensor_copy(out=XP[:, :, :, cc::16], in_=Z4[:, 0:2, :].rearrange("p r (q s) -> p r q s", s=2)), reads=["Z4"], writes=["XP"])
                    p.op("pool", lambda e: e.tensor_tensor(out=rt[0][:], in0=c1v, in1=Z4[:, 0:2, :], op=ALU.mult), reads=["C12", "Z4"], writes=["rt0"])
                    p.op("pool", lambda e: e.tensor_tensor(out=rt[1][:], in0=c2v, in1=Z4[:, 1:3, :], op=ALU.mult), reads=["C12", "Z4"], writes=["rt1"])
                    p.op("pool", lambda e: e.tensor_tensor(out=rt[0][:], in0=rt[0][:], in1=rt[1][:], op=ALU.add), reads=["rt0", "rt1"], writes=["rt0"])
                    p.op("pool", lambda e, cc=cc: e.tensor_tensor(out=Z4[:].rearrange("p (a r) x -> p a r x", a=2),
                                                                   in0=rt[0][:].unsqueeze(1).to_broadcast([128, 2, 2, 32]),
                                                                   in1=XLs[:, :, cc, :].unsqueeze(1).to_broadcast([128, 2, 2, 32]), op=ALU.add),
                         reads=["rt0", "XLs", "XP"], writes=["Z4"])
                for ch in range(4):
                    bk, bkey = nb()

                    def mmy(e, ch=ch, bk=bk):
                        uf = us[:, ch].rearrange("p j c -> p (j c)")
                        e.matmul(bk[:], lhsT=KL[:, ch, 0, :], rhs=uf, start=True, stop=False, skip_group_check=True)
                        e.matmul(bk[:], lhsT=DG[:, ch, :], rhs=uf, start=False, stop=False, skip_group_check=True)
                        for kk in range(1, 16):
                            e.matmul(bk[:, kk * 32:512], lhsT=KL[:, ch, kk, :], rhs=uf[:, 0:(16 - kk) * 32], start=False, stop=False, skip_group_check=True)
                        for a in range(4):
                            q = 4 * ch + a
                            for j in range(16):
                                for ri in range(2):
                                    ins = e.matmul(bk[32 * a:32 * a + 32, j * 32:(j + 1) * 32], lhsT=GS[:, q, j, ri, :], rhs=XP[:, ri, q, :], start=False,
                                                   stop=(a == 3 and j == 15 and ri == 1), skip_group_check=True, tile_position=(0, 32 * a))
                        return ins
                    p.op("pe", mmy, reads=["KL", "DG", "GS", "us", "XP"], writes=[bkey])
                    p.op("act", lambda e, ch=ch, bk=bk: e.activation(out=zb[:, ch, :].rearrange("p (c j) -> p c j", j=16), in_=bk[:].rearrange("p (j c) -> p c j", c=32),
                                                                     func=AF.Gelu_apprx_tanh), reads=[bkey], writes=["zb"])
                dump("XP", XP, "XP"); dump("zb", zb, "zb")
                wvv, wvk = load_block("w_val", 4, 0, 1024)
                wgv, wgk = load_block("w_gg", 4, 0, 1024)
                for mo in range(8):
                    bv, bvk = nb(); bg, bgk = nb()

                    def mmv(e, mo=mo, bv=bv):
                        for kc in range(4):
                            ins = e.matmul(bv[:], lhsT=wvv[:, kc, mo * 128:(mo + 1) * 128], rhs=zb[:, kc, :], start=(kc == 0), stop=(kc == 3))
                        return ins

                    def mmg(e, mo=mo, bg=bg):
                        for kc in range(4):
                            ins = e.matmul(bg[:], lhsT=wgv[:, kc, mo * 128:(mo + 1) * 128], rhs=zb[:, kc, :], start=(kc == 0), stop=(kc == 3))
                        return ins
                    if mo % 4 == 0:
                        wav, wak = load_block("w_in", 8, 1024 + (mo // 4) * 512, 512)
                    ba, bak = nb()

                    def mma(e, mo=mo, ba=ba, wav=wav):
                        for kc in range(8):
                            ins = e.matmul(ba[:], lhsT=wav[:, kc, (mo % 4) * 128:(mo % 4 + 1) * 128], rhs=xb[:, kc, :], start=(kc == 0), stop=(kc == 7))
                        return ins
                    p.op("pe", mmv, reads=[wvk, "zb"], writes=[bvk])
                    p.op("pe", mmg, reads=[wgk, "zb"], writes=[bgk])
                    p.op("pe", mma, reads=[wak, "xb"], writes=[bak])
                    t1, t1k = nt_(); t2, t2k = nt_(); t3, t3k = nt_()
                    p.op("act", lambda e, bg=bg, t1=t1: e.activation(out=t1[:], in_=bg[:], func=AF.Sigmoid), reads=[bgk], writes=[t1k])
                    p.op("act", lambda e, ba=ba, t3=t3: e.activation(out=t3[:], in_=ba[:], func=AF.Sigmoid), reads=[bak], writes=[t3k])
                    p.op("dve", lambda e, bv=bv, t1=t1, t2=t2: e.tensor_tensor(out=t2[:], in0=bv[:], in1=t1[:], op=ALU.mult), reads=[bvk, t1k], writes=[t2k])
                    p.op("dve", lambda e, t2=t2, t3=t3: e.tensor_tensor(out=t2[:], in0=t2[:], in1=t3[:], op=ALU.mult), reads=[t2k, t3k], writes=[t2k])
                    p.op("pool", lambda e, mo=mo, t2=t2: e.tensor_tensor(out=merged[:, mo, :], in0=merged[:, mo, :], in1=t2[:], op=ALU.add), reads=[t2k, "merged"], writes=["merged"])
                dump("zb", plb, "zb"); dump("mixb", mixb, "mixb"); dump("merged", merged, "merged")
                for cb in range(2):
                    wv, wk = load_block("w_out", 8, cb * 512, 512)
                    for mi in range(4):
                        mo = cb * 4 + mi
                        bk, bkey = nb()

                        def mmo(e, wv=wv, mi=mi, bk=bk):
                            for kc in range(8):
                                ins = e.matmul(bk[:], lhsT=wv[:, kc, mi * 128:(mi + 1) * 128], rhs=merged[:, kc, :], start=(kc == 0), stop=(kc == 7))
                            return ins
                        p.op("pe", mmo, reads=[wk, "merged"], writes=[bkey])
                        p.op("dve", lambda e, mo=mo, bk=bk: e.scalar_tensor_tensor(out=x32[:, mo, :], in0=x32[:, mo, :], scalar=ALPHA, in1=bk[:], op0=ALU.mult, op1=ALU.add),
                             reads=[bkey, kx], writes=[kx])


            def tail(t, part):
                tok0 = t * NT
                first = (t == 0)
                x32 = x32_[t % 2]
                kx = "x32_%d" % (t % 2)
                if part == "a":
                    dump("pre1", x32, kx)
                    layer_norm(x32, kx, "ln1g", "ln1b", merged, "merged", "a")
                    return
                if part == "b":
                    layer_norm(x32, kx, "ln1g", "ln1b", merged, "merged", "b")
                    p.dma("act", [lambda e, h=h, tok0=tok0: e.dma_start(out=h1T[4 * h:4 * h + 4, :, tok0:tok0 + NT].rearrange("k p n -> p k n"), in_=x32[:, 4 * h:4 * h + 4, :])
                                  for h in range(2)], reads=[kx], writes=["h1T_w%d" % t], key="h1T_%d" % (t % 2))
                    return
                bk, bkey = nb()

                def mmr(e, bk=bk):
                    for blk in range(4):
                        for kc in range(8):
                            ins = e.matmul(bk[:, blk * 32:(blk + 1) * 32], lhsT=x32[:, kc, blk * 128:(blk + 1) * 128], rhs=cst["w_r"][:, kc, :],
                                           start=(kc == 0), stop=(kc == 7), skip_group_check=True)
                    return ins
                p.op("pe", mmr, reads=[kx, "cst"], writes=[bkey])
                v3 = lambda ap: ap.rearrange("p (b e) -> p b e", e=32)
                p.op("dve", lambda e, bk=bk: e.tensor_tensor(out=lg[:], in0=v3(bk[:, 0:128]),
                                                             in1=cst["b_r"][:].unsqueeze(1).to_broadcast([128, 4, 32]), op=ALU.add), reads=[bkey, "cst"], writes=["lg"])
                for blk in range(4):
                    hT, hk = hT16[blk % 2][:].bitcast(BF16), "tmp%d" % (1 + blk % 2)
                    for half in range(2):
                        bk, bkey = nb()

                        def mmT(e, bk=bk, blk=blk, half=half):
                            for j in range(4):
                                ins = e.transpose(bk[:, j * 128:(j + 1) * 128], x32[:, half * 4 + j, blk * 128:(blk + 1) * 128], cst["ident"][:])
                            return ins
                        p.op("pe", mmT, reads=[kx, "cst"], writes=[bkey])
                        p.op("act", lambda e, bk=bk, hT=hT, half=half: e.activation(out=hT[:, half * 512:(half + 1) * 512], in_=bk[:], func=AF.Copy), reads=[bkey], writes=[hk])
                    p.dma("act", [lambda e, hT=hT, r0=tok0 + blk * 128: e.dma_start(out=h1tm16[r0:r0 + 128, :], in_=hT)],
                          reads=[hk], writes=["h1tm_%d" % (4 * t + blk)], key="h1tm16_%d" % (blk % 2))

                for blk in range(4):
                    p.op("dve", lambda e, blk=blk: e.max(out=mx8[:, blk, :], in_=lg[:, blk, :]), reads=["lg"], writes=["mx8"])
                for blk in range(4):
                    p.op("dve", lambda e, blk=blk: e.tensor_scalar(out=msk[:, blk, :], in0=lg[:, blk, :], scalar1=mx8[:, blk, 3:4], scalar2=None, op0=ALU.is_ge), reads=["lg", "mx8"], writes=["msk"])
                p.op("dve", lambda e: e.tensor_scalar(out=nmx[:], in0=mx8[:, :, 0], scalar1=-1.0, scalar2=None, op0=ALU.mult), reads=["mx8"], writes=["nmx"])
                for blk in range(4):
                    p.op("act", lambda e, blk=blk: e.activation(out=lg[:, blk, :], in_=lg[:, blk, :], func=AF.Exp, bias=nmx[:, blk:blk + 1], scale=1.0), reads=["lg", "nmx", "msk"], writes=["lg"])
                p.op("dve", lambda e: e.tensor_mul(out=lg[:], in0=lg[:], in1=msk[:]), reads=["lg", "msk"], writes=["lg"])
                p.op("dve", lambda e: e.reduce_sum(out=ssum[:], in_=lg[:], axis=AX.X), reads=["lg"], writes=["ssum"])
                p.op("dve", lambda e: e.reciprocal(out=ssum[:], in_=ssum[:]), reads=["ssum"], writes=["ssum"])
                p.op("dve", lambda e: e.tensor_tensor(out=lg[:], in0=lg[:], in1=ssum[:].unsqueeze(2).to_broadcast([128, 4, 32]), op=ALU.mult), reads=["lg", "ssum"], writes=["lg"])
                p.op("dve", lambda e: e.tensor_copy(out=lgw[:], in_=lg[:]), reads=["lg"], writes=["lgw"])
                deferred.append(lambda tok0=tok0, t=t: p.dma("pool", [lambda e, tok0=tok0: e.dma_start(out=wtab[tok0:tok0 + NT, :].rearrange("(b p) e -> p b e", p=128), in_=lgw[:])],
                                                             reads=["lgw"], writes=["wtab_%d" % t], key="wtab"))
                p.op("dve", lambda e: e.tensor_copy(out=msk16[:], in_=msk[:]), reads=["msk"], writes=["msk16"])
                bk, bkey = nb()

                def mmc(e, bk=bk):
                    for b in range(4):
                        ins = e.matmul(bk[:, b * 32:(b + 1) * 32], lhsT=TRI[:], rhs=msk16[:, b, :], start=True, stop=True, skip_group_check=True)
                    for b in range(1, 5):
                        for b2 in range(b):
                            ins = e.matmul(bk[:, 128 + b * 32:128 + (b + 1) * 32], lhsT=onesb[:], rhs=msk16[:, b2, :], start=(b2 == 0), stop=(b2 == b - 1), skip_group_check=True)
                    return ins
                p.op("pe", mmc, reads=["msk16", "TRI", "onesb"], writes=[bkey])
                p.op("dve", lambda e, bk=bk: e.tensor_tensor(out=pos[:], in0=v3(bk[:, 0:128]), in1=CNT[:].unsqueeze(1).to_broadcast([128, 4, 32]), op=ALU.add), reads=[bkey, "CNT"], writes=["pos"])
                p.op("dve", lambda e, bk=bk: e.tensor_tensor(out=pos[:, 1:4, :], in0=pos[:, 1:4, :], in1=v3(bk[:, 160:256]), op=ALU.add), reads=[bkey, "pos"], writes=["pos"])
                p.op("dve", lambda e, bk=bk: e.tensor_tensor(out=CNT[:], in0=CNT[:], in1=bk[:, 256:288], op=ALU.add), reads=[bkey, "CNT", "pos"], writes=["CNT"])
                p.op("dve", lambda e: e.tensor_single_scalar(out=gq[:], in_=pos[:], scalar=float(cap), op=ALU.is_ge), reads=["pos"], writes=["gq"])
                p.op("dve", lambda e: e.tensor_scalar(out=gq[:], in0=gq[:], scalar1=1.0e6, scalar2=None, op0=ALU.mult), reads=["gq"], writes=["gq"])
                p.op("dve", lambda e: e.tensor_add(out=pos[:], in0=pos[:], in1=gq[:]), reads=["pos", "gq"], writes=["pos"])
                p.op("dve", lambda e: e.tensor_tensor(out=pos[:], in0=pos[:], in1=cst["eoff1"][:].unsqueeze(1).to_broadcast([128, 4, 32]), op=ALU.add), reads=["pos", "cst"], writes=["pos"])
                p.op("dve", lambda e: e.tensor_mul(out=pos[:], in0=pos[:], in1=msk[:]), reads=["pos", "msk"], writes=["pos"])
                p.op("dve", lambda e: e.tensor_scalar(out=pos[:], in0=pos[:], scalar1=-1.0, scalar2=None, op0=ALU.add), reads=["pos"], writes=["pos"])
                for blk in range(4):
                    p.op("dve", lambda e, blk=blk: e.max(out=d8[:, blk, :], in_=pos[:, blk, :]), reads=["pos"], writes=["d8"])
                p.op("dve", lambda e: e.tensor_scalar(out=d4[:], in0=d8[:, :, 0:4], scalar1=float(NSLOT), scalar2=None, op0=ALU.min), reads=["d8"], writes=["d4"])
                p.op("dve", lambda e, t=t: e.tensor_copy(out=DEST[:, 4 * t:4 * t + 4, :], in_=d4[:]), reads=["d4"], writes=["DEST%d" % t])
                for blk in range(4):
                    gb = 4 * t + blk
                    for kk in range(4):
                        deferred.append(lambda gb=gb, kk=kk, t=t: p.dma("pool", [lambda e, gb=gb, kk=kk: e.indirect_dma_start(
                            out=toklist[:, :], out_offset=bass.IndirectOffsetOnAxis(ap=DEST[:, gb, kk:kk + 1], axis=0),
                            in_=TOKID[:, gb:gb + 1], in_offset=None, bounds_check=bcr(e, NSLOT - 1), oob_is_err=False)],
                            reads=["DEST%d" % t, "TOKID", "init"], writes=["tl_%d_%d" % (gb, kk)], key="toklist"))
            deferred = []

            def flush_scatters():
                for f_ in deferred:
                    f_()
                del deferred[:]
            front(0, "a")
            front(0, "b")
            for t in range(ntiles):
                mid(t)
                flush_scatters()
                tail(t, "a")
                if t + 1 < ntiles:
                    front(t + 1, "a")
                tail(t, "b")
                if t + 1 < ntiles:
                    front(t + 1, "b")
                tail(t, "c")
            flush_scatters()
            p.seal("toklist", ["toklist"])
            p.seal("h1tm16_0", ["h1tm16"])
            p.seal("h1tm16_1", ["h1tm16b"])
            p.seal("h1T_0", ["h1T"])
            p.seal("h1T_1", ["h1Tb"])
            p.seal("wtab", ["wtab"])

        p.barrier()
        with ExitStack() as esB:
            TB = lambda n, s, d: T(n, s, d, esB)
            NEB = 4
            ebuf = [TB("ebuf%d" % i, [128, 8, 1024], BF16) for i in range(NEB)]
            eb_i = [0]

            def load_expert(name, ex):
                i = eb_i[0] % NEB
                eb_i[0] += 1
                buf = ebuf[i]
                p.dma("pool", [lambda e, h=h: e.dma_start(out=buf[:, 2 * h:2 * h + 2, :], in_=D[name][ex, 2 * h:2 * h + 2].rearrange("k p n -> p k n")) for h in range(4)],
                      writes=["ebuf%d" % i])
                return buf, "ebuf%d" % i
            for name in ["b_eg", "b_eu"]:
                cst[name] = TB("c_" + name, [128, 32, 8], F32)
            p.dma("sync", [lambda e, name=name: e.dma_start(out=cst[name][:], in_=D[name]) for name in ["b_eg", "b_eu"]], writes=["cstB"], key="cstB")
            p.seal("cstB", ["cstB"])
            subs = [(s0, min(512, cap - s0)) for s0 in range(0, cap, 512)]
            Xfm = TB("Xfm", [128, 8, cap], BF16)
            Afm = TB("Afm", [128, 8, cap], BF16)
            NXB = 12
            xtm = [TB("xtm%d" % i, [128, 1024], BF16) for i in range(NXB)]
            tlb = [TB("tlb%d" % i, [128, NBLK], I32) for i in range(2)]
            wsl = [[TB("wsl%d_%d" % (i, b), [128, 32], F32) for b in range(NBLK)] for i in range(2)]
            bdr = [TB("bdr%d" % i, [1, 1024], F32) for i in range(2)]
            ones32 = TB("ones16", [1, 128], BF16)
            p.op("dve", lambda e: e.memset(ones32[:], 1.0), writes=["ones32"])
            bdb = [TB("bdb%d" % i, [1, 1024], BF16) for i in range(2)]
            ysb = [TB("ysb%d" % i, [128, 1024], F32) for i in range(3)]
            tmpB = [TB("tb%d" % i, [128, NT], F32) for i in range(6)]
            tb_i = [0]; xb_i = [0]; ys_i = [0]

            def ntb():
                i = tb_i[0] % 6
                tb_i[0] += 1
                return tmpB[i], "tb%d" % i

            def stage_in(ex):
                tl, tlk = tlb[ex % 2], "tlb%d" % (ex % 2)
                ws, wsk = wsl[ex % 2], "wsl%d" % (ex % 2)
                p.dma("sync", [lambda e: e.dma_start(out=tl[:], in_=toklist[ex * cap:(ex + 1) * cap, :].rearrange("(p b) o -> p (b o)", b=NBLK)),
                               lambda e: e.dma_start(out=bdr[ex % 2][:], in_=D["b_down"][ex:ex + 1, :])],
                      reads=["toklist"], writes=[tlk, "bdr%d" % (ex % 2)])
                p.op("dve", lambda e: e.tensor_copy(out=bdb[ex % 2][:], in_=bdr[ex % 2][:]), reads=["bdr%d" % (ex % 2)], writes=["bdb%d" % (ex % 2)])
                for b in range(NBLK):
                    i = xb_i[0] % NXB
                    xb_i[0] += 1
                    xt, xk = xtm[i], "xtm%d" % i
                    p.dma("pool", [lambda e, b=b, xt=xt: e.indirect_dma_start(out=xt[:, :], out_offset=None, in_=h1tm16[:, :],
                                                                               in_offset=bass.IndirectOffsetOnAxis(ap=tl[:, b:b + 1], axis=0),
                                                                               bounds_check=bcr(e, ntok), oob_is_err=False)],
                          reads=[tlk, "h1tm16", "h1tm16b", "init"], writes=[xk])
                    p.dma("pool", [lambda e, b=b: e.indirect_dma_start(out=ws[b][:, :], out_offset=None, in_=wtab[:, :],
                                                                       in_offset=bass.IndirectOffsetOnAxis(ap=tl[:, b:b + 1], axis=0),
                                                                       bounds_check=bcr(e, ntok), oob_is_err=False)],
                          reads=[tlk, "wtab", "init"], writes=[wsk + "_%d" % b], key=wsk)
                    bk, bkey = nb(8)
                    pbv = bk[:].bitcast(BF16).rearrange("p (k s) -> p k s", s=128)

                    def mmt(e, xt=xt, pbv=pbv):
                        for kc in range(8):
                            ins = e.transpose(pbv[:, kc, :], xt[:, kc * 128:(kc + 1) * 128], identb[:])
                        return ins
                    p.op("pe", mmt, reads=[xk, "identb"], writes=[bkey])
                    if b % 2 == 0:
                        p.op("dve", lambda e, b=b, pbv=pbv: e.tensor_copy(out=Xfm[:, :, b * 128:(b + 1) * 128], in_=pbv), reads=[bkey], writes=["Xfm"])
                    else:
                        p.op("act", lambda e, b=b, pbv=pbv: e.activation(out=Xfm[:, :, b * 128:(b + 1) * 128], in_=pbv, func=AF.Copy), reads=[bkey], writes=["Xfm"])
                p.seal(wsk, [wsk])

            wl = {}

            def prefetch(name, ex):
                if ex < n_experts:
                    wl[(name, ex)] = load_expert(name, ex)

            def stage_gu(ex):
                wgb, wgk = wl[("w_eg", ex)]
                wub, wuk = wl[("w_eu", ex)]
                for (s0, n) in subs:
                    for mo in range(8):
                        bg, bgk = nb(8); bu, buk = nb(8)

                        def mmg(e, mo=mo, bg=bg, wgb=wgb, s0=s0, n=n):
                            for kc in range(8):
                                ins = e.matmul(bg[:, 0:n], lhsT=wgb[:, kc, mo * 128:(mo + 1) * 128], rhs=Xfm[:, kc, s0:s0 + n], start=(kc == 0), stop=(kc == 7))
                            return ins

                        def mmu(e, mo=mo, bu=bu, wub=wub, s0=s0, n=n):
                            for kc in range(8):
                                ins = e.matmul(bu[:, 0:n], lhsT=wub[:, kc, mo * 128:(mo + 1) * 128], rhs=Xfm[:, kc, s0:s0 + n], start=(kc == 0), stop=(kc == 7))
                            return ins
                        p.op("pe", mmg, reads=[wgk, "Xfm"], writes=[bgk])
                        p.op("pe", mmu, reads=[wuk, "Xfm"], writes=[buk])
                        g_, gk = ntb(); sg, sgk = ntb(); u_, uk = ntb()
                        p.op("dve", lambda e, mo=mo, bg=bg, g_=g_, n=n: e.tensor_scalar(out=g_[:, 0:n], in0=bg[:, 0:n], scalar1=cst["b_eg"][:, ex, mo:mo + 1], scalar2=7.0, op0=ALU.add, op1=ALU.min),
                             reads=[bgk, "cstB"], writes=[gk])
                        p.op("act", lambda e, g_=g_, sg=sg, n=n: e.activation(out=sg[:, 0:n], in_=g_[:, 0:n], func=AF.Sigmoid, scale=1.702), reads=[gk], writes=[sgk])
                        p.op("act", lambda e, mo=mo, bu=bu, u_=u_, n=n: e.activation(out=u_[:, 0:n], in_=bu[:, 0:n], func=AF.Identity, bias=cst["b_eu"][:, ex, mo:mo + 1], scale=1.0),
                             reads=[buk, "cstB"], writes=[uk])
                        p.op("dve", lambda e, u_=u_, n=n: e.tensor_scalar(out=u_[:, 0:n], in0=u_[:, 0:n], scalar1=7.0, scalar2=-7.0, op0=ALU.min, op1=ALU.max), reads=[uk], writes=[uk])
                        p.op("dve", lambda e, u_=u_, g_=g_, n=n: e.scalar_tensor_tensor(out=u_[:, 0:n], in0=u_[:, 0:n], scalar=1.0, in1=g_[:, 0:n], op0=ALU.add, op1=ALU.mult), reads=[uk, gk], writes=[uk])
                        p.op("dve", lambda e, mo=mo, u_=u_, sg=sg, s0=s0, n=n: e.tensor_tensor(out=Afm[:, mo, s0:s0 + n], in0=u_[:, 0:n], in1=sg[:, 0:n], op=ALU.mult), reads=[uk, sgk], writes=["Afm"])

            def stage_down(ex):
                wdb, wdk = wl[("w_ed", ex)]
                ws, wsk = wsl[ex % 2], "wsl%d" % (ex % 2)
                bd_, bdk_ = bdb[ex % 2], "bdb%d" % (ex % 2)
                for b in range(NBLK):
                    i = ys_i[0] % 3
                    ys_i[0] += 1
                    yt, yk = ysb[i], "ysb%d" % i
                    for half in range(2):
                        bk, bkey = nb(8)

                        def mmd(e, b=b, half=half, bk=bk):
                            for kc in range(8):
                                e.matmul(bk[:], lhsT=Afm[:, kc, b * 128:(b + 1) * 128], rhs=wdb[:, kc, half * 512:(half + 1) * 512], start=(kc == 0), stop=False, skip_group_check=True)
                            return e.matmul(bk[:], lhsT=ones32[:], rhs=bd_[:, half * 512:(half + 1) * 512], start=False, stop=True, skip_group_check=True)
                        p.op("pe", mmd, reads=[wdk, "Afm", "ones32", bdk_], writes=[bkey])
                        p.op("act", lambda e, b=b, half=half, bk=bk, yt=yt: e.activation(out=yt[:, half * 512:(half + 1) * 512], in_=bk[:], func=AF.Copy, scale=ws[b][:, ex:ex + 1]),
                             reads=[bkey, wsk], writes=[yk])
                    p.dma("sync", [lambda e, yt=yt, b=b: e.dma_start(out=ysd[ex * cap:(ex + 1) * cap, :].rearrange("(p b) d -> p b d", b=NBLK)[:, b, :], in_=yt[:])],
                          reads=[yk], writes=["ys_%d_%d" % (ex, b)], key="ysd_%d" % i)

            prefetch("w_eg", 0)
            prefetch("w_eu", 0)
            stage_in(0)
            for ex in range(n_experts):
                prefetch("w_ed", ex)
                prefetch("w_eg", ex + 1)
                stage_gu(ex)
                if ex + 1 < n_experts:
                    stage_in(ex + 1)
                prefetch("w_eu", ex + 1)
                stage_down(ex)
            p.seal("ysd_0", ["ysd"])
            p.seal("ysd_1", ["ysdb"])
            p.seal("ysd_2", ["ysdc"])

        p.barrier()
        with ExitStack() as esC:
            TB = lambda n, s, d: T(n, s, d, esC)
            wplg = TB("wplg", [128, 8, 1024], BF16)
            wplp = TB("wplp", [128, 2, 1024], BF16)
            p.dma("sync", [lambda e: e.dma_start(out=wplg[:], in_=S["w_plg"].rearrange("k p n -> p k n")),
                           lambda e: e.dma_start(out=wplp[:], in_=S["w_plp"].rearrange("k p n -> p k n"))], reads=["prepA"], writes=["wpl"], key="wpl")
            p.seal("wpl", ["wpl"])
            h32_ = [TB("h32_%d" % i, [128, 8, NT], F32) for i in range(2)]
            hb_ = [TB("hb_%d" % i, [128, 8, NT], BF16) for i in range(2)]
            acc_ = [TB("acc_%d" % i, [128, 8, NT], F32) for i in range(2)]
            p32_ = [TB("p32_%d" % i, [128, 2, NT], F32) for i in range(2)]
            pb16_ = [TB("pb16_%d" % i, [128, 2, NT], BF16) for i in range(2)]
            ybuf = [TB("yb%d" % i, [128, 1024], F32) for i in range(12)]
            tmpB = [TB("tc%d" % i, [128, NT], F32) for i in range(6)]
            tb_i = [0]

            def ntb():
                i = tb_i[0] % 6
                tb_i[0] += 1
                return tmpB[i], "tc%d" % i
            lnm = TB("lnmB", [128, NT], F32); lnr = TB("lnrB", [128, NT], F32)
            sqb = TB("sqbB", [128, 8, NT], BF16)

            def layer_norm2(src, skey, gname, bname, xbf, xbkey):
                p.op("dve", lambda e: e.tensor_copy(out=xbf[:], in_=src[:]), reads=[skey], writes=[xbkey])
                p.op("act", lambda e: e.activation(out=sqb[:], in_=src[:], func=AF.Square), reads=[skey], writes=["sqbB"])
                b1, k1 = nb(8); b2, k2 = nb(8)

                def mm1(e):
                    for kc in range(8):
                        ins = e.matmul(b1[:], lhsT=onesb[:], rhs=xbf[:, kc, :], start=(kc == 0), stop=(kc == 7))
                    return ins

                def mm2(e):
                    for kc in range(8):
                        ins = e.matmul(b2[:], lhsT=onesb[:], rhs=sqb[:, kc, :], start=(kc == 0), stop=(kc == 7))
                    return ins
                p.op("pe", mm1, reads=[xbkey, "onesb"], writes=[k1])
                p.op("pe", mm2, reads=["sqbB", "onesb"], writes=[k2])
                p.op("act", lambda e: e.activation(out=lnm[:], in_=b1[:], func=AF.Copy, scale=1.0 / 1024), reads=[k1], writes=["lnmB"])
                p.op("dve", lambda e: e.tensor_mul(out=lnr[:], in0=lnm[:], in1=lnm[:]), reads=["lnmB"], writes=["lnrB"])
                p.op("dve", lambda e: e.scalar_tensor_tensor(out=lnr[:], in0=b2[:], scalar=1.0 / 1024, in1=lnr[:], op0=ALU.mult, op1=ALU.subtract), reads=[k2, "lnrB"], writes=["lnrB"])
                p.op("act", lambda e: e.activation(out=lnr[:], in_=lnr[:], func=AF.Sqrt, bias=1e-5, scale=1.0), reads=["lnrB"], writes=["lnrB"])
                p.op("dve", lambda e: e.reciprocal(out=lnr[:], in_=lnr[:]), reads=["lnrB"], writes=["lnrB"])
                bc8 = lambda t: t[:].unsqueeze(1).to_broadcast([128, 8, NT])
                p.op("dve", lambda e: e.tensor_tensor(out=src[:], in0=src[:], in1=bc8(lnm), op=ALU.subtract), reads=[skey, "lnmB"], writes=[skey])
                p.op("dve", lambda e: e.tensor_tensor(out=src[:], in0=src[:], in1=bc8(lnr), op=ALU.mult), reads=[skey, "lnrB"], writes=[skey])
                for kc in range(8):
                    p.op("act", lambda e, kc=kc: e.activation(out=src[:, kc, :], in_=src[:, kc, :], func=AF.Identity,
                                                              scale=cst[gname][:, kc:kc + 1], bias=cst[bname][:, kc:kc + 1]),
                         reads=[skey, "cst"], writes=[skey])

            ysum = {}

            def gather_block(t, blk):
                gb = 4 * t + blk
                ys_ = []
                for kk in range(4):
                    i = (gb * 4 + kk) % 12
                    yb, ybk = ybuf[i], "yb%d" % i
                    p.dma("pool", [lambda e, gb=gb, kk=kk, yb=yb: e.indirect_dma_start(out=yb[:, :], out_offset=None, in_=ysd[:, :],
                                                                                     in_offset=bass.IndirectOffsetOnAxis(ap=DEST[:, gb, kk:kk + 1], axis=0),
                                                                                     bounds_check=bcr(e, NSLOT), oob_is_err=False)],
                          reads=["ysd", "ysdb", "ysdc", "init"], writes=[ybk])
                    ys_.append((yb, ybk))
                ysum[(t, blk)] = ys_

            def combine_a(t):
                tok0 = t * NT
                h32, hb, acc, p32, pb16 = h32_[t % 2], hb_[t % 2], acc_[t % 2], p32_[t % 2], pb16_[t % 2]
                kh32, khb, kacc, kp32, kpb16 = ["%s_%d" % (n_, t % 2) for n_ in ("h32", "hb", "acc", "p32", "pb16")]
                p.dma("sync", [lambda e, h=h, tok0=tok0: e.dma_start(out=h32[:, 4 * h:4 * h + 4, :], in_=h1T[4 * h:4 * h + 4, :, tok0:tok0 + NT].rearrange("k p n -> p k n"))
                               for h in range(2)], reads=["h1T", "h1Tb"], writes=[kh32])
                p.dma("sync", [lambda e, tok0=tok0: e.dma_start(out=p32[:], in_=D["pT"][:, :, tok0:tok0 + NT].rearrange("k p n -> p k n"))], writes=[kp32])
                for blk in range(3):
                    gather_block(t, blk)

            def combine_b(t):
                tok0 = t * NT
                h32, hb, acc, p32, pb16 = h32_[t % 2], hb_[t % 2], acc_[t % 2], p32_[t % 2], pb16_[t % 2]
                kh32, khb, kacc, kp32, kpb16 = ["%s_%d" % (n_, t % 2) for n_ in ("h32", "hb", "acc", "p32", "pb16")]
                p.op("dve", lambda e: e.tensor_copy(out=hb[:], in_=h32[:]), reads=[kh32], writes=[khb])
                p.op("act", lambda e: e.activation(out=pb16[:], in_=p32[:], func=AF.Copy), reads=[kp32], writes=[kpb16])
                for blk in range(4):
                    ys_ = ysum[(t, blk)]
                    for half in range(2):
                        bk, bkey = nb(8)

                        def mmT2(e, bk=bk, ys_=ys_, half=half):
                            for j in range(4):
                                kc = half * 4 + j
                                for kk in range(4):
                                    ins = e.matmul(bk[:, j * 128:(j + 1) * 128], lhsT=ys_[kk][0][:, kc * 128:(kc + 1) * 128], rhs=cst["ident"][:],
                                                   start=(kk == 0), stop=(kk == 3), skip_group_check=True)
                            return ins
                        p.op("pe", mmT2, reads=[k_ for _, k_ in ys_] + ["cst"], writes=[bkey])
                        p.op("act", lambda e, bk=bk, half=half, blk=blk: e.activation(out=acc[:, 4 * half:4 * half + 4, blk * 128:(blk + 1) * 128],
                                                                                        in_=bk[:].rearrange("p (j s) -> p j s", s=128), func=AF.Copy), reads=[bkey], writes=[kacc])
                    if blk == 0:
                        gather_block(t, 3)

            def ple_ln(t):
                tok0 = t * NT
                h32, hb, acc, p32, pb16 = h32_[t % 2], hb_[t % 2], acc_[t % 2], p32_[t % 2], pb16_[t % 2]
                kh32, khb, kacc, kp32, kpb16 = ["%s_%d" % (n_, t % 2) for n_ in ("h32", "hb", "acc", "p32", "pb16")]
                for mo in range(8):
                    bg, bgk = nb(8); bp, bpk = nb(8)

                    def mmpg(e, mo=mo, bg=bg):
                        for kc in range(8):
                            ins = e.matmul(bg[:], lhsT=wplg[:, kc, mo * 128:(mo + 1) * 128], rhs=hb[:, kc, :], start=(kc == 0), stop=(kc == 7))
                        return ins

                    def mmpp(e, mo=mo, bp=bp):
                        for kc in range(2):
                            ins = e.matmul(bp[:], lhsT=wplp[:, kc, mo * 128:(mo + 1) * 128], rhs=pb16[:, kc, :], start=(kc == 0), stop=(kc == 1))
                        return ins
                    p.op("pe", mmpg, reads=["wpl", khb], writes=[bgk])
                    p.op("pe", mmpp, reads=["wpl", kpb16], writes=[bpk])
                    sg, sgk = ntb(); t1, t1k = ntb()
                    p.op("act", lambda e, bg=bg, sg=sg: e.activation(out=sg[:], in_=bg[:], func=AF.Sigmoid), reads=[bgk], writes=[sgk])
                    p.op("dve", lambda e, bp=bp, sg=sg, t1=t1: e.tensor_tensor(out=t1[:], in0=bp[:], in1=sg[:], op=ALU.mult), reads=[bpk, sgk], writes=[t1k])
                    p.op("dve", lambda e, mo=mo, t1=t1: e.tensor_tensor(out=t1[:], in0=acc[:, mo, :], in1=t1[:], op=ALU.add), reads=[t1k, kacc], writes=[t1k])
                    p.op("dve", lambda e, mo=mo, t1=t1: e.scalar_tensor_tensor(out=h32[:, mo, :], in0=h32[:, mo, :], scalar=ALPHA, in1=t1[:], op0=ALU.mult, op1=ALU.add),
                         reads=[t1k, kh32], writes=[kh32])
                layer_norm2(h32, kh32, "ln2g", "ln2b", hb, khb)
                p.dma("sync", [lambda e, h=h, tok0=tok0: e.dma_start(out=outT[4 * h:4 * h + 4, :, tok0:tok0 + NT].rearrange("k p n -> p k n"), in_=h32[:, 4 * h:4 * h + 4, :])
                               for h in range(2)], reads=[kh32], writes=["outT_%d" % (t % 2)])
            combine_a(0)
            combine_b(0)
            for t in range(ntiles):
                if t + 1 < ntiles:
                    combine_a(t + 1)
                ple_ln(t)
                if t + 1 < ntiles:
                    combine_b(t + 1)
            p.final_wait("sync", ["outT_0", "outT_1", "h1T", "h1Tb"] + ["dbg_" + n for n in dbg_outs])
            p.final_wait("pool", ["prepA"])

        p.emit(block)
    return nc


def shared_layout(I, cap=1536):
    f = lambda a: np.ascontiguousarray(a, dtype=np.float32)
    out = {}
    out["w_in"] = f(I["w_in"][0].reshape(8, 128, 3072))
    out["w_val"] = f(I["w_glu_val"][0].reshape(4, 128, 1024))
    out["w_gg"] = f(I["w_glu_gate"][0].reshape(4, 128, 1024))
    out["w_pp"] = f(I["w_pool_proj"][0].reshape(4, 128, 1024))
    out["w_out"] = f(I["w_out"][0].reshape(8, 128, 1024))
    out["w_plg"] = f(I["w_ple_gate"][0].reshape(8, 128, 1024))
    out["w_plp"] = f(I["w_ple_proj"][0].reshape(2, 128, 1024))
    out["w_pg"] = f(I["w_pool_group"][0].transpose(1, 0, 2))
    out["pscale"] = f(I["pool_scale"][0].reshape(4, 128).T)
    out["ln1g"] = f(I["ln1_g"][0].reshape(8, 128).T)
    out["ln1b"] = f(I["ln1_b"][0].reshape(8, 128).T)
    out["ln2g"] = f(I["ln2_g"][0].reshape(8, 128).T)
    out["ln2b"] = f(I["ln2_b"][0].reshape(8, 128).T)
    out["w_r"] = f(I["w_router"][0].reshape(8, 128, 32).transpose(1, 0, 2))
    out["b_r"] = f(np.broadcast_to(I["b_router"][0][None, :], (128, 32)))
    out["w_eg"] = f(I["w_gate"][0].reshape(32, 8, 128, 1024))
    out["w_eu"] = f(I["w_up"][0].reshape(32, 8, 128, 1024))
    out["w_ed"] = f(I["w_down"][0].reshape(32, 8, 128, 1024))
    for n, s in [("b_eg", "b_gate"), ("b_eu", "b_up")]:
        out[n] = f(I[s][0].reshape(32, 8, 128).transpose(2, 0, 1))
    out["b_down"] = f(I["b_down"][0])
    lr, li, ls = I["ssm_lambda_re"][0], I["ssm_lambda_im"][0], I["ssm_log_step"][0]
    br, bi = I["ssm_b_re"][0], I["ssm_b_im"][0]
    cr, ci = I["ssm_c_re"][0], I["ssm_c_im"][0]
    dd = I["ssm_d"][0]
    g_S = (2 * np.arange(16)[None, :] + np.arange(2)[:, None])
    lrS = lr[g_S]
    out["lrS"] = f(lrS.transpose(0, 2, 1).reshape(128, 16))
    out["liS"] = f(li[g_S].transpose(0, 2, 1).reshape(128, 16))
    out["lsS"] = f(np.broadcast_to(ls[g_S][:, :, None], (2, 16, 64)).transpose(0, 2, 1).reshape(128, 16))

    def padS(arr_gph):
        o = np.zeros((2, 64, 16, 2, 16), np.float32)
        for m in range(2):
            o[m, :, :, m, :] = arr_gph[g_S[m]].transpose(1, 0, 2)
        return o.reshape(128, 16, 32)
    out["cSr"] = padS(cr.transpose(0, 2, 1)); out["cSi"] = padS(ci.transpose(0, 2, 1))
    out["bSr"] = padS(br); out["bSi"] = padS(bi)
    a_, ch_, m_ = np.arange(4), np.arange(4), np.arange(2)
    gT = 2 * (4 * ch_[None, :, None] + a_[:, None, None]) + m_[None, None, :]

    def repT(arr_gp):
        v = arr_gp[gT]
        v = np.broadcast_to(v[:, None, None], (4, 2, 16, 4, 2, 64))
        return f(v.reshape(128, 4, 128))
    out["lrT"] = repT(lr); out["liT"] = repT(li)
    out["lsT"] = repT(np.broadcast_to(ls[:, None], (32, 64)))

    def padT(arr_gph):
        o = np.zeros((4, 2, 16, 4, 2, 64), np.float32)
        for a in range(4):
            for ch in range(4):
                for m in range(2):
                    g = 2 * (4 * ch + a) + m
                    o[a, m, :, ch, m, :] = arr_gph[g].T
        return o.reshape(128, 4, 128)
    out["bTr"] = padT(br); out["bTi"] = padT(bi)
    dT = np.zeros((4, 2, 16, 4), np.float32)
    for a in range(4):
        for ch in range(4):
            for m in range(2):
                dT[a, m, :, ch] = dd[2 * (4 * ch + a) + m]
    out["dT"] = dT.reshape(128, 4)
    out["ident"] = np.eye(128, dtype=np.float32)
    out["tri"] = np.triu(np.ones((128, 128), np.float32), 1)
    pos = np.arange(1, 17, dtype=np.float32)
    invc = np.stack([1.0 / np.minimum(pos, float(w)) for w in (2, 4, 8, 16)], 0)
    out["invc"] = f(np.broadcast_to(invc[None], (128, 4, 16)))
    out["eoff1"] = f(np.broadcast_to((np.arange(32, dtype=np.float32) * cap + 1.0)[None], (128, 32)))
    return out


def core_inputs(x_c, p_c):
    ns, L = x_c.shape[0], x_c.shape[1]
    ntok = ns * L
    perm = lambda a: a.reshape(ns, L // 256, 256, a.shape[-1]).transpose(1, 0, 2, 3).reshape(ntok, a.shape[-1])
    xT = np.ascontiguousarray(perm(x_c).T).reshape(8, 128, ntok)
    pT = np.ascontiguousarray(perm(p_c).T).reshape(2, 128, ntok)
    return {"xT": xT, "pT": pT}


def core_output(oT, ns, L):
    o = np.ascontiguousarray(oT.reshape(1024, ns * L).T)
    return np.ascontiguousarray(o.reshape(L // 256, ns, 256, 1024).transpose(1, 0, 2, 3)).reshape(ns, L, 1024)


def kernel(**inputs):
    I = {k: np.asarray(v) for k, v in inputs.items()}
    x, pp = I["x"], I["p"][0]
    B, L, Dm = x.shape
    ncore = 8
    nseq = B // ncore
    shared = shared_layout(I)
    nc = build(nseq, L)
    in_maps = []
    for c in range(ncore):
        m = dict(shared)
        m.update(core_inputs(x[c * nseq:(c + 1) * nseq], pp[c * nseq:(c + 1) * nseq]))
        in_maps.append(m)
    res = run_bass_kernel_spmd(nc, in_maps, core_ids=list(range(ncore)))
    outs = []
    for c in range(ncore):
        outs.append(core_output(np.asarray(res.results[c]["outT"]), nseq, L))
    return np.concatenate(outs, axis=0).astype(np.float32)
```
